# Optimizing a Trainium2 kernel written in Bass

```python
import math
import jax, jax.numpy as jnp
from jax import lax
import numpy as np

D_MODEL = 2048
BATCH = 1
SEQ = 8192
DEPTH = 2

HEAD_DIM = 128
D_MIX = D_MODEL
N_DIFF_HEADS = 8
DIFF_QK_DIM = HEAD_DIM // 2
N_SB_HEADS = 8
D_DIFF = N_DIFF_HEADS * HEAD_DIM
D_SB = N_SB_HEADS * HEAD_DIM
D_IN = 3 * D_DIFF + 3 * D_SB
N_EXPERTS = 32
TOP_K = 4
D_FF = D_MODEL
SWIGLU_LIMIT = 7.0
SWIGLU_ALPHA = 1.702
ROPE_THETA = 10000.0
Q_BLOCK = 128
TOKEN_BLOCK = 128
RMS_EPS = 1e-5
N_MOD = 6

kernel_name = "hybrid_diffattn_stickbreak_moe_adaln"


def rmsnorm(x, w, eps=RMS_EPS):
    xf = x.astype(jnp.float32)
    y = xf * lax.rsqrt(jnp.mean(xf * xf, axis=-1, keepdims=True) + eps)
    return (y * w.astype(jnp.float32)).astype(x.dtype)


def lambda_init_fn(layer_idx):
    return 0.8 - 0.6 * math.exp(-0.3 * layer_idx)


def rope(x, positions):
    d = x.shape[-1]
    half = d // 2
    inv_freq = ROPE_THETA ** (-jnp.arange(half, dtype=jnp.float32) / half)
    ang = positions.astype(jnp.float32)[..., None] * inv_freq
    ang = ang.reshape(ang.shape[:2] + (1,) * (x.ndim - 3) + (half,))
    cos, sin = jnp.cos(ang), jnp.sin(ang)
    xf = x.astype(jnp.float32)
    x1, x2 = xf[..., :half], xf[..., half:]
    return jnp.concatenate([x1 * cos - x2 * sin, x2 * cos + x1 * sin], axis=-1)


def diff_attention(q, k, v, lam):
    B, S, H = v.shape[:3]
    nb = S // Q_BLOCK
    scale = DIFF_QK_DIM ** -0.5
    qh = q.transpose(0, 2, 3, 1, 4).astype(jnp.float32) * scale
    kh = k.transpose(0, 2, 3, 1, 4).astype(jnp.float32)
    vh = v.transpose(0, 2, 1, 3).astype(jnp.float32)
    kidx = jnp.arange(S)

    def block(i):
        qb = lax.dynamic_slice_in_dim(qh, i * Q_BLOCK, Q_BLOCK, axis=3)
        s = jnp.einsum('bhpqd,bhpkd->bhpqk', qb, kh)
        qidx = i * Q_BLOCK + jnp.arange(Q_BLOCK)
        causal = kidx[None, :] <= qidx[:, None]
        p = jax.nn.softmax(jnp.where(causal, s, -jnp.inf), axis=-1)
        a = p[:, :, 0] - lam * p[:, :, 1]
        return jnp.einsum('bhqk,bhkd->bhqd', a, vh)

    out = lax.map(block, jnp.arange(nb))
    return out.transpose(1, 2, 0, 3, 4).reshape(B, H, S, HEAD_DIM)


def stick_breaking_attention(q, k, v):
    B, S, H, d = q.shape
    nb = S // Q_BLOCK
    scale = d ** -0.5
    qh = q.transpose(0, 2, 1, 3).astype(jnp.float32) * scale
    kh = k.transpose(0, 2, 1, 3).astype(jnp.float32)
    vh = v.transpose(0, 2, 1, 3).astype(jnp.float32)
    kidx = jnp.arange(S)

    def block(i):
        qb = lax.dynamic_slice_in_dim(qh, i * Q_BLOCK, Q_BLOCK, axis=2)
        z = jnp.einsum('bhqd,bhkd->bhqk', qb, kh)
        qidx = i * Q_BLOCK + jnp.arange(Q_BLOCK)
        strict = kidx[None, :] < qidx[:, None]
        log_1m = jnp.where(strict, jax.nn.log_sigmoid(-z), 0.0)
        rem = lax.cumsum(log_1m, axis=3, reverse=True) - log_1m
        a = jnp.where(strict, jnp.exp(jax.nn.log_sigmoid(z) + rem), 0.0)
        return jnp.einsum('bhqk,bhkd->bhqd', a, vh)

    out = lax.map(block, jnp.arange(nb))
    return out.transpose(1, 2, 0, 3, 4).reshape(B, H, S, d)


def moe_ffn(h, router_w, router_b, w_gate_up, b_gate_up, w_down, b_down):
    B, S, D = h.shape
    n = B * S
    ht = h.reshape(n, D)
    logits = jnp.matmul(ht, router_w).astype(jnp.float32) + router_b.astype(jnp.float32)
    top_v, top_i = lax.top_k(logits, TOP_K)
    gates = jax.nn.softmax(top_v, axis=-1)
    combine = jnp.sum(jax.nn.one_hot(top_i, N_EXPERTS, dtype=jnp.float32) * gates[..., None], axis=1)

    def token_block(args):
        xc, cc = args
        gu = jnp.einsum('nd,edf->nef', xc, w_gate_up) + b_gate_up
        gate = jnp.minimum(gu[..., 0::2], SWIGLU_LIMIT)
        up = jnp.clip(gu[..., 1::2], -SWIGLU_LIMIT, SWIGLU_LIMIT)
        act = gate * jax.nn.sigmoid(SWIGLU_ALPHA * gate) * (up + 1.0)
        y = jnp.einsum('nef,efd->ned', act, w_down) + b_down
        return jnp.einsum('ne,ned->nd', cc.astype(y.dtype), y)

    out = lax.map(token_block, (ht.reshape(-1, TOKEN_BLOCK, D), combine.reshape(-1, TOKEN_BLOCK, N_EXPERTS)))
    return out.reshape(B, S, D).astype(h.dtype)


def setup_inputs(seed: int = 0) -> dict:
    key = jax.random.key(seed)
    ks = jax.random.split(key, 24)
    f32 = jnp.float32
    nrm = lambda k, shape, s: jax.random.normal(k, shape, f32) * s
    x = jax.random.normal(ks[0], (BATCH, SEQ, D_MODEL), f32)
    c = jax.random.normal(ks[1], (BATCH, D_MODEL), f32)
    steps = jax.random.randint(ks[2], (BATCH, SEQ), 1, 3, dtype=jnp.int32)
    positions = (jnp.cumsum(steps, axis=1) - 1).astype(jnp.int32)
    return {
        "x": x,
        "c": c,
        "positions": positions,
        "ada_w": nrm(ks[3], (DEPTH, D_MODEL, N_MOD * D_MODEL), 0.5 * D_MODEL ** -0.5),
        "ada_b": nrm(ks[4], (DEPTH, N_MOD * D_MODEL), 0.02),
        "norm_mix": 1.0 + nrm(ks[5], (DEPTH, D_MODEL), 0.02),
        "w_in": nrm(ks[6], (DEPTH, D_MODEL, D_IN), D_MODEL ** -0.5),
        "w_out": nrm(ks[7], (DEPTH, D_MIX, D_MODEL), D_MIX ** -0.5),
        "lambda_q1": nrm(ks[8], (DEPTH, DIFF_QK_DIM), 0.1),
        "lambda_k1": nrm(ks[9], (DEPTH, DIFF_QK_DIM), 0.1),
        "lambda_q2": nrm(ks[10], (DEPTH, DIFF_QK_DIM), 0.1),
        "lambda_k2": nrm(ks[11], (DEPTH, DIFF_QK_DIM), 0.1),
        "subln_w": 1.0 + nrm(ks[12], (DEPTH, HEAD_DIM), 0.02),
        "sb_norm_w": 1.0 + nrm(ks[13], (DEPTH, HEAD_DIM), 0.02),
        "norm_ffn": 1.0 + nrm(ks[14], (DEPTH, D_MODEL), 0.02),
        "router_w": nrm(ks[15], (DEPTH, D_MODEL, N_EXPERTS), D_MODEL ** -0.5),
        "router_b": nrm(ks[16], (DEPTH, N_EXPERTS), 0.01),
        "w_gate_up": nrm(ks[17], (DEPTH, N_EXPERTS, D_MODEL, 2 * D_FF), D_MODEL ** -0.5),
        "b_gate_up": nrm(ks[18], (DEPTH, N_EXPERTS, 2 * D_FF), 0.02),
        "w_down": nrm(ks[19], (DEPTH, N_EXPERTS, D_FF, D_MODEL), D_FF ** -0.5),
        "b_down": nrm(ks[20], (DEPTH, N_EXPERTS, D_MODEL), 0.02),
        "norm_final": 1.0 + nrm(ks[21], (D_MODEL,), 0.02),
    }


def reference(x, c, positions, ada_w, ada_b, norm_mix, w_in, w_out, lambda_q1, lambda_k1, lambda_q2, lambda_k2,
              subln_w, sb_norm_w, norm_ffn, router_w, router_b, w_gate_up, b_gate_up, w_down, b_down, norm_final):
    B, S, D = x.shape
    c_act = jax.nn.silu(c)
    for l in range(DEPTH):
        lam_init = lambda_init_fn(l)
        mod = jnp.matmul(c_act, ada_w[l]) + ada_b[l]
        shift_a, scale_a, gate_a, shift_f, scale_f, gate_f = [m[:, None, :] for m in jnp.split(mod, N_MOD, axis=-1)]

        h = rmsnorm(x, norm_mix[l]) * (1.0 + scale_a) + shift_a
        proj = jnp.matmul(h, w_in[l])
        dq, dk, dv, sq, sk, sv = jnp.split(proj, [D_DIFF, 2 * D_DIFF, 3 * D_DIFF, 3 * D_DIFF + D_SB, 3 * D_DIFF + 2 * D_SB], axis=-1)

        dq = rope(dq.reshape(B, S, N_DIFF_HEADS, 2, DIFF_QK_DIM), positions)
        dk = rope(dk.reshape(B, S, N_DIFF_HEADS, 2, DIFF_QK_DIM), positions)
        dv = dv.reshape(B, S, N_DIFF_HEADS, HEAD_DIM)
        lam = (jnp.exp(jnp.sum(lambda_q1[l].astype(jnp.float32) * lambda_k1[l].astype(jnp.float32)))
               - jnp.exp(jnp.sum(lambda_q2[l].astype(jnp.float32) * lambda_k2[l].astype(jnp.float32)))
               + lam_init)
        a_out = diff_attention(dq, dk, dv, lam)
        a_out = rmsnorm(a_out, subln_w[l]) * (1.0 - lam_init)
        a_out = a_out.transpose(0, 2, 1, 3).reshape(B, S, D_DIFF)

        b_out = stick_breaking_attention(sq.reshape(B, S, N_SB_HEADS, HEAD_DIM),
                                         sk.reshape(B, S, N_SB_HEADS, HEAD_DIM),
                                         sv.reshape(B, S, N_SB_HEADS, HEAD_DIM))
        b_out = rmsnorm(b_out, sb_norm_w[l]).transpose(0, 2, 1, 3).reshape(B, S, D_SB)

        mix = jnp.concatenate([a_out.astype(x.dtype), b_out.astype(x.dtype)], axis=-1)
        x = x + gate_a * jnp.matmul(mix, w_out[l])

        h = rmsnorm(x, norm_ffn[l]) * (1.0 + scale_f) + shift_f
        x = x + gate_f * moe_ffn(h, router_w[l], router_b[l], w_gate_up[l], b_gate_up[l], w_down[l], b_down[l])
    return rmsnorm(x, norm_final)
```

```python
import math
import numpy as np
import ml_dtypes
import concourse.bass as bass
import concourse.mybir as mybir
from concourse.bass_utils import run_bass_kernel_spmd

F32 = mybir.dt.float32
BF16 = mybir.dt.bfloat16
I32 = mybir.dt.int32
ALU = mybir.AluOpType
AF = mybir.ActivationFunctionType
AX = mybir.AxisListType
NPBF = ml_dtypes.bfloat16

NCORES = 8
S = 8192
D = 2048
NCH = 16
TG = 512
NG = S // TG
EPS = 1e-5
SAME_ENGINE_SYNC = True
SEM_LIMIT = 30000


def _region(ap):
    t = ap.tensor
    name = t.name
    pairs = ap.ap
    off = int(ap.offset)
    if type(t).__name__.startswith('DRam'):
        hi = off
        for st, cnt in pairs:
            hi += abs(st) * (cnt - 1)
        return (name, 0, 0, off, hi)
    row = 1
    for s in t.shape[1:]:
        row *= s
    p0 = off // row
    f0 = off % row
    p1 = p0
    f1 = f0
    for st, cnt in pairs:
        if st >= row and st % row == 0:
            p1 += (st // row) * (cnt - 1)
        else:
            f1 += abs(st) * (cnt - 1)
    return (name, p0, p1, f0, f1)


class Op:
    __slots__ = ('eng', 'fn', 'idx', 'deps', 'dma_waits', 'signal', 'dma_key', 'inc')

    def __init__(self, eng, fn):
        self.eng = eng
        self.fn = fn
        self.deps = {}
        self.dma_waits = {}
        self.signal = False
        self.dma_key = None
        self.inc = 16


class Prog:
    ENG = ('pe', 'act', 'dve', 'pool', 'sp')

    def __init__(self, nc):
        self.nc = nc
        self.ops = {e: [] for e in self.ENG}
        self.tiles = {}
        self.dma_cnt = {}
        self.dma_sem = {}
        self.nsem = 0

    def _newsem(self, nm):
        self.nsem += 1
        return self.nc.alloc_semaphore(f"s{self.nsem}_{nm}")

    def _dep(self, op, op2):
        if op2 is op:
            return
        if op2.dma_key is not None:
            k = op2.dma_key
            op.dma_waits[k] = max(op.dma_waits.get(k, 0), self.dma_cnt[k])
            return
        if op2.eng == op.eng and op.dma_key is None:
            if op.eng == 'pe' or not SAME_ENGINE_SYNC:
                return
        if op.deps.get(op2.eng, -1) < op2.idx:
            op.deps[op2.eng] = op2.idx

    def add(self, eng, fn, reads=(), writes=(), dma_key=None, inc=16):
        op = Op(eng, fn)
        op.idx = len(self.ops[eng])
        op.dma_key = dma_key
        op.inc = inc
        self.ops[eng].append(op)
        accs = [(_region(a), False) for a in reads] + [(_region(a), True) for a in writes]
        for (r, w) in accs:
            lst = self.tiles.get(r[0])
            if lst is None:
                continue
            psum = r[0].startswith('ps')
            for e in lst:
                if psum and e[4].eng != eng:
                    self._dep(op, e[4])
                elif (w or e[5]) and not (e[1] < r[1] or e[0] > r[2] or e[3] < r[3] or e[2] > r[4]):
                    self._dep(op, e[4])
        if dma_key is not None:
            self.dma_cnt[dma_key] = self.dma_cnt.get(dma_key, 0) + inc
        for (r, w) in accs:
            lst = self.tiles.setdefault(r[0], [])
            if w:
                lst[:] = [e for e in lst if not (e[0] >= r[1] and e[1] <= r[2] and e[2] >= r[3] and e[3] <= r[4])]
            else:
                lst[:] = [e for e in lst if not ((not e[5]) and e[4].eng == eng and e[4].dma_key is None
                                                 and dma_key is None
                                                 and e[0] >= r[1] and e[1] <= r[2] and e[2] >= r[3] and e[3] <= r[4])]
            lst.append([r[1], r[2], r[3], r[4], op, w])
        return op

    def dma(self, eng, out, in_, key=None, **kw):
        if key is None:
            od = type(out.tensor).__name__.startswith('DRam')
            idr = type(in_.tensor).__name__.startswith('DRam')
            key = out.tensor.name if (not od or idr) else in_.tensor.name
        return self.add(eng, lambda e: e.dma_start(out=out, in_=in_, **kw), reads=[in_], writes=[out], dma_key=key)

    def barrier(self):
        lasts = {}
        for e in self.ENG:
            j = len(self.ops[e]) - 1
            while j >= 0 and self.ops[e][j].dma_key is not None:
                j -= 1
            lasts[e] = j
        cnts = dict(self.dma_cnt)
        for e in self.ENG:
            op = Op(e, lambda eng: eng.nop())
            op.idx = len(self.ops[e])
            self.ops[e].append(op)
            for e2, j in lasts.items():
                if j >= 0 and e2 != e:
                    op.deps[e2] = j
            for k, c in cnts.items():
                op.dma_waits[k] = c
        self.tiles = {}

    def emit(self):
        nc = self.nc
        for e in self.ENG:
            for op in self.ops[e]:
                for e2, j in op.deps.items():
                    self.ops[e2][j].signal = True
        tick = {}
        for e in self.ENG:
            sem = None
            cnt = SEM_LIMIT + 1
            lst = []
            for op in self.ops[e]:
                if op.signal:
                    if cnt >= SEM_LIMIT:
                        sem = self._newsem(e)
                        cnt = 0
                    cnt += 1
                    lst.append((sem, cnt))
                else:
                    lst.append(None)
            tick[e] = lst
        for k in self.dma_cnt:
            self.dma_sem[k] = self._newsem('d')

        def run(e, eng):
            waited = {}
            for i, op in enumerate(self.ops[e]):
                for e2, j in op.deps.items():
                    sem, v = tick[e2][j]
                    key = id(sem)
                    if waited.get(key, 0) >= v:
                        continue
                    waited[key] = v
                    eng.wait_ge(sem, v)
                for k, c in op.dma_waits.items():
                    sem = self.dma_sem[k]
                    key = id(sem)
                    if waited.get(key, 0) >= c:
                        continue
                    waited[key] = c
                    eng.wait_ge(sem, c)
                inst = op.fn(eng)
                if op.dma_key is not None:
                    inst.then_inc(self.dma_sem[op.dma_key], op.inc)
                elif op.signal:
                    s, v = tick[e][i]
                    inst.then_inc(s, 1)

        with nc.Block() as block:
            @block.tensor
            def _(eng):
                run('pe', eng)

            @block.scalar
            def _(eng):
                run('act', eng)

            @block.vector
            def _(eng):
                run('dve', eng)

            @block.gpsimd
            def _(eng):
                run('pool', eng)

            @block.sync
            def _(eng):
                run('sp', eng)


class K:
    def __init__(self, nc):
        self.nc = nc
        self.P = Prog(nc)
        self.n = 0

    def sb(self, shape, dt, name=None):
        self.n += 1
        return self.nc.alloc_sbuf_tensor(name or f"sb{self.n}", list(shape), dt).ap()

    def ps(self, shape=(128, 512), dt=F32, name=None):
        self.n += 1
        return self.nc.alloc_psum_tensor(name or f"ps{self.n}", list(shape), dt).ap()

    def mm(self, out, lhsT, rhs, start=True, stop=True):
        self.P.add('pe', lambda e: e.matmul(out, lhsT, rhs, start=start, stop=stop), reads=[lhsT, rhs], writes=[out])

    def act(self, out, in_, func, bias=None, scale=1.0, accum_out=None):
        reads = [in_]
        kw = {}
        if bias is not None:
            kw['bias'] = bias
            if not isinstance(bias, (int, float)):
                reads.append(bias)
        if not isinstance(scale, (int, float)):
            reads.append(scale)
        writes = [out]
        if accum_out is not None:
            kw['accum_out'] = accum_out
            writes.append(accum_out)
        self.P.add('act', lambda e: e.activation(out=out, in_=in_, func=func, scale=scale, **kw), reads=reads, writes=writes)

    def ts(self, eng, out, in0, s1, s2, op0, op1=None):
        reads = [in0] + [s for s in (s1, s2) if s is not None and not isinstance(s, (int, float))]
        if op1 is None:
            self.P.add(eng, lambda e: e.tensor_scalar(out, in0, s1, None, op0), reads=reads, writes=[out])
        else:
            self.P.add(eng, lambda e: e.tensor_scalar(out, in0, s1, s2, op0, op1), reads=reads, writes=[out])

    def tt(self, eng, out, in0, in1, op):
        self.P.add(eng, lambda e: e.tensor_tensor(out, in0, in1, op), reads=[in0, in1], writes=[out])

    def stt(self, eng, out, in0, scalar, in1, op0, op1):
        reads = [in0, in1] + ([] if isinstance(scalar, (int, float)) else [scalar])
        self.P.add(eng, lambda e: e.scalar_tensor_tensor(out, in0, scalar, in1, op0, op1), reads=reads, writes=[out])

    def copy(self, eng, out, in_):
        self.P.add(eng, lambda e: e.tensor_copy(out, in_), reads=[in_], writes=[out])

    def recip(self, out, in_):
        self.P.add('dve', lambda e: e.reciprocal(out, in_), reads=[in_], writes=[out])

    def memset(self, eng, ap, v):
        self.P.add(eng, lambda e: e.memset(ap, v), writes=[ap])

    def dma(self, eng, out, in_, **kw):
        self.P.dma(eng, out, in_, **kw)

    def finish(self):
        self.P.barrier()
        self.P.emit()


def col16(v):
    return np.ascontiguousarray(np.asarray(v, np.float32).reshape(NCH, 128).T)


MCOL = 12288 // NCORES
MCH = MCOL // 128


def build_M():
    nc = bass.Bass("TRN2", target_bir_lowering=False)
    k = K(nc)
    cvec = nc.dram_tensor("cvec", [128, NCH], F32, kind="ExternalInput").ap()
    w = nc.dram_tensor("w", [2, D, MCOL], F32, kind="ExternalInput").ap()
    b = nc.dram_tensor("b", [128, 2 * MCH], F32, kind="ExternalInput").ap()
    out = nc.dram_tensor("out", [128, 2 * MCH], F32, kind="ExternalOutput").ap()
    c_sb = k.sb([128, NCH], F32)
    ca = k.sb([128, NCH], F32)
    b_sb = k.sb([128, 2 * MCH], F32)
    o_sb = k.sb([128, 2 * MCH], F32)
    w_sb = [k.sb([128, NCH, MCOL // 2], F32) for _ in range(2)]
    ps = k.ps([128, 2 * MCH])
    k.dma('sp', c_sb, cvec)
    k.dma('sp', b_sb, b)
    k.act(ca, c_sb, AF.Silu)
    for l in range(2):
        for hf in range(2):
            wt = w_sb[hf]
            k.dma('sp', wt, w[l].rearrange("(c p) n -> p c n", p=128)[:, :, hf * (MCOL // 2):(hf + 1) * (MCOL // 2)])
            for jj in range(MCH // 2):
                j = l * MCH + hf * (MCH // 2) + jj
                for c in range(NCH):
                    k.mm(ps[:, j:j + 1], wt[:, c, jj * 128:(jj + 1) * 128], ca[:, c:c + 1], start=(c == 0), stop=(c == NCH - 1))
    k.tt('dve', o_sb, ps, b_sb, ALU.add)
    k.dma('sp', out, o_sb)
    k.finish()
    return nc


def run_M(inp):
    nc = build_M()
    in_maps = []
    for c in range(NCORES):
        sl = slice(c * MCOL, (c + 1) * MCOL)
        bb = np.concatenate([inp['ada_b'][l][sl].reshape(MCH, 128).T for l in range(2)], axis=1)
        in_maps.append({
            "cvec": col16(inp['c'][0]),
            "w": np.ascontiguousarray(inp['ada_w'][:, :, sl]),
            "b": np.ascontiguousarray(bb.astype(np.float32)),
        })
    res = run_bass_kernel_spmd(nc, in_maps, core_ids=list(range(NCORES)))
    mod = np.zeros((2, 12288), np.float32)
    for c in range(NCORES):
        o = res.results[c]["out"]
        for l in range(2):
            mod[l, c * MCOL:(c + 1) * MCOL] = o[:, l * MCH:(l + 1) * MCH].T.reshape(-1)
    return mod


def ab_consts():
    c = {}
    p = np.arange(128)
    inv = (10000.0 ** (-(np.arange(32, dtype=np.float32)) / 32)).astype(np.float32)
    c['invf'] = (inv[p % 32].astype(np.float64) / (2 * np.pi)).astype(np.float32).reshape(128, 1)
    rot = np.zeros((128, 128), np.float32)
    for m in range(128):
        if (m % 64) < 32:
            rot[m + 32, m] = -1.0
        else:
            rot[m - 32, m] = 1.0
    c['rot'] = rot.astype(NPBF)
    c['ones'] = np.ones((128, 128), NPBF)
    kk = np.arange(128)[:, None]
    qq = np.arange(512)[None, :]
    c['maskd'] = np.stack([((128 * j + kk) <= qq) for j in range(4)], 1).astype(NPBF)
    c['masks'] = np.stack([((128 * j + kk) < qq) for j in range(4)], 1).astype(NPBF)
    jj = np.arange(128)[:, None]
    ss = np.arange(128)[None, :]
    c['negtri'] = (-(jj >= ss).astype(np.float32)).astype(NPBF)
    c['negones'] = (-np.ones((1, 128), np.float32)).astype(NPBF)
    return c


def build_AB(lam_init, ng=NG, stop=None):
    nc = bass.Bass("TRN2", target_bir_lowering=False)
    k = K(nc)
    St = ng * TG
    nkb = St // 128
    xT = nc.dram_tensor("xT", [D, St], F32, kind="ExternalInput").ap()
    posb = nc.dram_tensor("posb", [128, St], I32, kind="ExternalInput").ap()
    win = nc.dram_tensor("win", [D, 768], F32, kind="ExternalInput").ap()
    vecs = nc.dram_tensor("vecs", [128, 3 * NCH], F32, kind="ExternalInput").ap()
    lamv = nc.dram_tensor("lamv", [128, 4 * 64], F32, kind="ExternalInput").ap()
    hw = nc.dram_tensor("hw", [128, 2], F32, kind="ExternalInput").ap()
    invf_d = nc.dram_tensor("invf", [128, 1], F32, kind="ExternalInput").ap()
    rot_d = nc.dram_tensor("rot", [128, 128], BF16, kind="ExternalInput").ap()
    ones_d = nc.dram_tensor("ones", [128, 128], BF16, kind="ExternalInput").ap()
    maskd_d = nc.dram_tensor("maskd", [128, 4, 512], BF16, kind="ExternalInput").ap()
    masks_d = nc.dram_tensor("masks", [128, 4, 512], BF16, kind="ExternalInput").ap()
    negtri_d = nc.dram_tensor("negtri", [128, 128], BF16, kind="ExternalInput").ap()
    negones_d = nc.dram_tensor("negones", [1, 128], BF16, kind="ExternalInput").ap()
    mixd = nc.dram_tensor("mixd", [128, St], BF16, kind="ExternalOutput").ap()
    mixs = nc.dram_tensor("mixs", [128, St], BF16, kind="ExternalOutput").ap()

    win_sb = k.sb([128, NCH, 768], BF16)
    vec_sb = k.sb([128, 3 * NCH], F32)
    wa = k.sb([128, NCH], F32)
    lam_sb = k.sb([128, 256], F32)
    hw_sb = k.sb([128, 2], F32)
    invf = k.sb([128, 1], F32)
    rot = k.sb([128, 128], BF16)
    ones = k.sb([128, 128], BF16)
    maskd = k.sb([128, 4, 512], BF16)
    masks = k.sb([128, 4, 512], BF16)
    negtri = k.sb([128, 128], BF16)
    negones = k.sb([1, 128], BF16)
    qd = k.sb([128, St], BF16)
    kd = k.sb([128, St], BF16)
    qs = k.sb([128, St], BF16)
    ks = k.sb([128, St], BF16)
    vd = k.sb([128, nkb, 128], BF16)
    vs = k.sb([128, nkb, 128], BF16)
    neglam = k.sb([128, 1], F32)
    wsub = k.sb([128, 1], F32)

    k.dma('pool', win_sb, win.rearrange("(c p) n -> p c n", p=128))
    for dst, src in ((vec_sb, vecs), (lam_sb, lamv), (hw_sb, hw), (invf, invf_d), (rot, rot_d), (ones, ones_d),
                     (maskd, maskd_d), (masks, masks_d), (negtri, negtri_d), (negones, negones_d)):
        k.dma('sp', dst, src)
    k.stt('dve', wa, vec_sb[:, NCH:2 * NCH], 1.0, vec_sb[:, 2 * NCH:3 * NCH], ALU.add, ALU.mult)
    sha = vec_sb[:, 0:NCH]
    lt = k.sb([128, 128], F32)
    l12 = k.sb([128, 2], F32)
    k.tt('dve', lt[:, 0:64], lam_sb[:, 0:64], lam_sb[:, 64:128], ALU.mult)
    k.tt('dve', lt[:, 64:128], lam_sb[:, 128:192], lam_sb[:, 192:256], ALU.mult)
    k.P.add('dve', lambda e: e.reduce_sum(l12[:, 0:1], lt[:, 0:64], AX.X), reads=[lt[:, 0:64]], writes=[l12[:, 0:1]])
    k.P.add('dve', lambda e: e.reduce_sum(l12[:, 1:2], lt[:, 64:128], AX.X), reads=[lt[:, 64:128]], writes=[l12[:, 1:2]])
    k.act(l12, l12, AF.Exp)
    k.tt('dve', neglam, l12[:, 1:2], l12[:, 0:1], ALU.subtract)
    k.ts('dve', neglam, neglam, -float(lam_init), None, ALU.add)
    k.ts('dve', wsub, hw_sb[:, 0:1], float(1.0 - lam_init), None, ALU.mult)
    sbw = hw_sb[:, 1:2]

    if stop == 'S':
        k.finish()
        return nc
    xg = k.sb([128, NCH, TG], F32)
    hT = k.sb([128, NCH, TG], BF16)
    sqb = [k.sb([128, TG], BF16) for _ in range(2)]
    tmpb = [k.sb([128, TG], F32) for _ in range(2)]
    rstd = k.sb([128, TG], F32)
    posi = k.sb([128, TG], I32)
    posf = k.sb([128, TG], F32)
    uc = k.sb([128, TG], F32)
    rr = k.sb([128, TG], F32)
    cosT = k.sb([128, TG], F32)
    sinT = k.sb([128, TG], F32)
    xsb = [k.sb([128, TG], BF16) for _ in range(2)]
    t1 = k.sb([128, TG], F32)
    t2 = k.sb([128, TG], F32)
    pbank = [k.ps() for _ in range(8)]

    for g in range(ng):
        tsl = slice(g * TG, (g + 1) * TG)
        k.dma('sp', xg, xT.rearrange("(c p) t -> p c t", p=128)[:, :, tsl])
        k.dma('sp', posi, posb[:, tsl])
        ssq = pbank[0]
        for c in range(NCH):
            k.act(sqb[c % 2], xg[:, c, :], AF.Square)
            k.mm(ssq, ones, sqb[c % 2], start=(c == 0), stop=(c == NCH - 1))
        k.act(rstd, ssq, AF.Sqrt, bias=EPS, scale=1.0 / D)
        k.recip(rstd, rstd)
        for c in range(NCH):
            tb = tmpb[c % 2]
            k.stt('dve', tb, xg[:, c, :], wa[:, c:c + 1], rstd, ALU.mult, ALU.mult)
            k.act(hT[:, c, :], tb, AF.Identity, bias=sha[:, c:c + 1])
        k.copy('dve', posf, posi)
        k.ts('dve', uc, posf, invf[:, 0:1], None, ALU.mult)
        for (tab, shift) in ((sinT, 0.0), (cosT, 0.25)):
            if shift:
                k.ts('dve', rr, uc, shift, None, ALU.add)
                src = rr
            else:
                src = uc
            k.ts('dve', t1, src, 12582912.0, 12582912.0, ALU.add, ALU.subtract)
            k.tt('dve', t2, src, t1, ALU.subtract)
            k.act(tab, t2, AF.Sin, scale=2.0 * math.pi)
        for o in range(4):
            pb = pbank[1 + o]
            for c in range(NCH):
                k.mm(pb, win_sb[:, c, o * 128:(o + 1) * 128], hT[:, c, :], start=(c == 0), stop=(c == NCH - 1))
        for t in range(TG // 128):
            pv = pbank[5 + (t % 2)]
            for c in range(NCH):
                k.mm(pv[:, 0:256], hT[:, c, t * 128:(t + 1) * 128], win_sb[:, c, 512:768], start=(c == 0), stop=(c == NCH - 1))
            kb = g * (TG // 128) + t
            k.copy('dve', vd[:, kb, :], pv[:, 0:128])
            k.act(vs[:, kb, :], pv[:, 128:256], AF.Copy)
        k.act(qs[:, tsl], pbank[3], AF.Copy, scale=float(128 ** -0.5))
        k.act(ks[:, tsl], pbank[4], AF.Copy)
        for i, (pb, dst, sc) in enumerate(((pbank[1], qd, 0.125), (pbank[2], kd, 1.0))):
            k.act(xsb[i], pb, AF.Copy)
            rp = pbank[7]
            k.mm(rp, rot, xsb[i])
            k.tt('dve', t1, xsb[i], cosT, ALU.mult)
            k.tt('dve', t2, rp, sinT, ALU.mult)
            k.stt('dve', dst[:, tsl], t1, sc, t2, ALU.mult, ALU.add) if sc == 1.0 else \
                k.stt('dve', t1, t1, 1.0, t2, ALU.mult, ALU.add)
            if sc != 1.0:
                k.ts('dve', dst[:, tsl], t1, sc, None, ALU.mult)

    if stop == 'A':
        k.finish()
        return nc
    pT = [[hT[:, 0, :], hT[:, 1, :]], [hT[:, 2, :], hT[:, 3, :]]]
    ez = [sinT, t1]
    sp = [hT[:, 4, :], hT[:, 5, :]]
    aT = [hT[:, 6, :], hT[:, 7, :]]
    Rsb = [hT[0:1, 8, :], hT[0:1, 9, :]]
    o1 = rstd
    o2 = posf
    rd = uc
    af = rr
    sqa = hT[:, 10, :]
    rs2 = cosT
    outb = [hT[:, 11, :], hT[:, 12, :]]

    def head_norm_out(src_f32, wcol, dst_dram, tsl, bank, ob):
        k.act(sqa, src_f32, AF.Square)
        k.mm(bank, ones, sqa)
        k.act(rs2, bank, AF.Sqrt, bias=EPS, scale=1.0 / 128)
        k.recip(rs2, rs2)
        k.stt('dve', ob, src_f32, wcol, rs2, ALU.mult, ALU.mult)
        k.dma('sp', dst_dram[:, tsl], ob)

    A1 = [pbank[0], pbank[1]]
    A2 = [pbank[2], pbank[3]]
    O1, D1, O2, D2 = pbank[4], pbank[5], pbank[6], pbank[7]
    for G in range(ng):
        tsl = slice(G * TG, (G + 1) * TG)
        nb = 4 * (G + 1)

        def st1(b):
            par = b % 2
            ksl = slice(b * 128, (b + 1) * 128)
            k.mm(A1[par], kd[0:64, ksl], qd[0:64, tsl])
            k.mm(A2[par], kd[64:128, ksl], qd[64:128, tsl])
            k.act(pT[par][0], A1[par], AF.Exp)
            k.act(pT[par][1], A2[par], AF.Exp)
            jj = b - 4 * G
            if jj >= 0:
                k.tt('pool', pT[par][0], pT[par][0], maskd[:, jj, :], ALU.mult)
                k.tt('pool', pT[par][1], pT[par][1], maskd[:, jj, :], ALU.mult)

        def st2(b):
            par = b % 2
            first = (b == 0)
            last = (b == nb - 1)
            k.mm(O1, vd[:, b, :], pT[par][0], start=first, stop=last)
            k.mm(D1, ones, pT[par][0], start=first, stop=last)
            k.mm(O2, vd[:, b, :], pT[par][1], start=first, stop=last)
            k.mm(D2, ones, pT[par][1], start=first, stop=last)

        for i in range(nb + 1):
            if i < nb:
                st1(i)
            if i >= 1:
                st2(i - 1)
        k.recip(rd, D1)
        k.tt('dve', o1, O1, rd, ALU.mult)
        k.recip(rd, D2)
        k.tt('dve', o2, O2, rd, ALU.mult)
        k.stt('dve', af, o2, neglam[:, 0:1], o1, ALU.mult, ALU.add)
        head_norm_out(af, wsub[:, 0:1], mixd, tsl, A1[0], outb[G % 2])

    if stop == 'B1':
        k.finish()
        return nc
    Az = [pbank[0], pbank[1]]
    Bz = [pbank[2], pbank[3]]
    Rn = [pbank[4], pbank[5]]
    Os = pbank[6]
    ssb = pbank[7]
    for G in range(ng):
        tsl = slice(G * TG, (G + 1) * TG)
        nb = 4 * (G + 1)
        order = list(range(nb - 1, -1, -1))

        def s1(i):
            b = order[i]
            par = i % 2
            ksl = slice(b * 128, (b + 1) * 128)
            k.mm(Az[par], ks[:, ksl], qs[:, tsl])
            k.act(ez[par], Az[par], AF.Exp)
            k.act(sp[par], ez[par], AF.Ln, bias=1.0)
            jj = b - 4 * G
            if jj >= 0:
                k.tt('pool', sp[par], sp[par], masks[:, jj, :], ALU.mult)

        def s2(i):
            b = order[i]
            par = i % 2
            ksl = slice(b * 128, (b + 1) * 128)
            first = (i == 0)
            k.mm(Bz[par], ks[:, ksl], qs[:, tsl], start=True, stop=False)
            k.mm(Bz[par], negtri, sp[par], start=False, stop=first)
            if not first:
                k.mm(Bz[par], negones[0:1, :], Rsb[(i - 1) % 2][0:1, :], start=False, stop=True)
            if i < nb - 1:
                k.mm(Rn[par][0:1, :], ones[:, 0:1], sp[par], start=True, stop=first)
                if not first:
                    k.mm(Rn[par][0:1, :], ones[0:1, 0:1], Rsb[(i - 1) % 2][0:1, :], start=False, stop=True)
                k.copy('dve', Rsb[i % 2][0:1, :], Rn[par][0:1, :])
            k.act(aT[par], Bz[par], AF.Exp)
            jj = b - 4 * G
            if jj >= 0:
                k.tt('pool', aT[par], aT[par], masks[:, jj, :], ALU.mult)
            k.mm(Os, vs[:, b, :], aT[par], start=first, stop=(i == nb - 1))

        for i in range(nb + 1):
            if i < nb:
                s1(i)
            if i >= 1:
                s2(i - 1)
        k.act(af, Os, AF.Copy)
        head_norm_out(af, sbw, mixs, tsl, ssb, outb[G % 2])
    k.finish()
    return nc


def lam_init_fn(l):
    return 0.8 - 0.6 * math.exp(-0.3 * l)


def run_AB(inp, l, xT, mod_l, ng=NG, stop=None):
    St = ng * TG
    nc = build_AB(lam_init_fn(l), ng=ng, stop=stop)
    cst = ab_consts()
    sha, sca = mod_l[0:D], mod_l[D:2 * D]
    vecs = np.concatenate([col16(sha), col16(sca), col16(inp['norm_mix'][l])], axis=1)
    lamv = np.concatenate([inp['lambda_q1'][l], inp['lambda_k1'][l], inp['lambda_q2'][l], inp['lambda_k2'][l]])
    lamv = np.ascontiguousarray(np.broadcast_to(lamv[None, :], (128, 256)).astype(np.float32))
    hw = np.ascontiguousarray(np.stack([inp['subln_w'][l], inp['sb_norm_w'][l]], axis=1).astype(np.float32))
    posb = np.ascontiguousarray(np.broadcast_to(inp['positions'][0][None, :St], (128, St)).astype(np.int32))
    w_in = inp['w_in'][l]
    xTs = np.ascontiguousarray(xT[:, :St])
    in_maps = []
    for c in range(NCORES):
        cols = []
        for base in (0, 1024, 3072, 4096, 2048, 5120):
            cols.append(w_in[:, base + c * 128: base + (c + 1) * 128])
        m = {"xT": xTs, "posb": posb, "win": np.ascontiguousarray(np.concatenate(cols, axis=1)),
             "vecs": np.ascontiguousarray(vecs), "lamv": lamv, "hw": hw}
        m.update(cst)
        in_maps.append(m)
    res = run_bass_kernel_spmd(nc, in_maps, core_ids=list(range(NCORES)))
    mixT = np.zeros((D, St), NPBF)
    for c in range(NCORES):
        mixT[c * 128:(c + 1) * 128] = res.results[c]["mixd"]
        mixT[1024 + c * 128:1024 + (c + 1) * 128] = res.results[c]["mixs"]
    return mixT


NEL = 4


def build_CD(ng=NG, nown=2):
    nc = bass.Bass("TRN2", target_bir_lowering=False)
    k = K(nc)
    St = ng * TG
    xT = nc.dram_tensor("xT", [D, St], F32, kind="ExternalInput").ap()
    mixT = nc.dram_tensor("mixT", [D, St], BF16, kind="ExternalInput").ap()
    wout_t = nc.dram_tensor("wout_t", [4, 128, 8192], F32, kind="ExternalInput").ap()
    vecs = nc.dram_tensor("vecs", [128, 4 * NCH], F32, kind="ExternalInput").ap()
    wr_d = nc.dram_tensor("wr", [128, NCH * 32], F32, kind="ExternalInput").ap()
    rb_d = nc.dram_tensor("rb", [128, 32], F32, kind="ExternalInput").ap()
    wgu_t = nc.dram_tensor("wgu_t", [NEL, 8, 128, 8192], F32, kind="ExternalInput").ap()
    bgu_d = nc.dram_tensor("bgu", [128, NEL * 32], F32, kind="ExternalInput").ap()
    wd_t = nc.dram_tensor("wd_t", [NEL, 4, 128, 8192], F32, kind="ExternalInput").ap()
    bd_d = nc.dram_tensor("bd", [1, NEL * D], F32, kind="ExternalInput").ap()
    ones_d = nc.dram_tensor("ones", [128, 128], BF16, kind="ExternalInput").ap()
    id_d = nc.dram_tensor("ident", [32, 32], F32, kind="ExternalInput").ap()
    x1T = nc.dram_tensor("x1T", [D, nown * TG], F32, kind="ExternalOutput").ap()
    y = nc.dram_tensor("y", [St, D], F32, kind="ExternalOutput").ap()
    wgu_bf = [[nc.dram_tensor(f"wgubf{e}_{s}", [128, 8192], BF16).ap() for s in range(8)] for e in range(NEL)]
    wd_bf = [[nc.dram_tensor(f"wdbf{e}_{q}", [128, 8192], BF16).ap() for q in range(4)] for e in range(NEL)]
    wout_bf = [nc.dram_tensor(f"woutbf{q}", [128, 8192], BF16).ap() for q in range(4)]

    def cast(dst, src):
        k.dma('pool', dst.rearrange("p (a b) -> p a b", b=2048), src.rearrange("p (a b) -> p a b", b=2048))

    for q in range(4):
        cast(wout_bf[q], wout_t[q])
    for e in range(NEL):
        for s in range(8):
            cast(wgu_bf[e][s], wgu_t[e, s])
        for q in range(4):
            cast(wd_bf[e][q], wd_t[e, q])

    vec_sb = k.sb([128, 4 * NCH], F32)
    wf = k.sb([128, NCH], F32)
    wr = k.sb([128, NCH, 32], F32)
    rb = k.sb([128, 32], F32)
    bgu = k.sb([128, NEL * 32], F32)
    ones = k.sb([128, 128], BF16)
    ident = k.sb([32, 32], F32)
    k.dma('sp', vec_sb, vecs)
    k.dma('sp', wr, wr_d.rearrange("p (c e) -> p c e", e=32))
    k.dma('sp', rb, rb_d)
    k.dma('sp', bgu, bgu_d)
    k.dma('sp', ones, ones_d)
    k.dma('sp', ident, id_d)
    ga = vec_sb[:, 0:NCH]
    shf = vec_sb[:, 2 * NCH:3 * NCH]
    k.stt('dve', wf, vec_sb[:, NCH:2 * NCH], 1.0, vec_sb[:, 3 * NCH:4 * NCH], ALU.add, ALU.mult)

    xg = k.sb([128, NCH, TG], F32)
    mg = k.sb([128, NCH, TG], BF16)
    hT = k.sb([128, NCH, TG], BF16)
    actT = k.sb([128, NCH, TG], BF16)
    yacc = k.sb([128, 4, D], F32)
    slots = [k.sb([128, NCH, 512], BF16) for _ in range(3)]
    sqb = [k.sb([128, TG], BF16) for _ in range(2)]
    tmpb = [k.sb([128, TG], F32) for _ in range(2)]
    h32 = [k.sb([128, TG], F32) for _ in range(2)]
    rstd = k.sb([128, TG], F32)
    g1 = [k.sb([128, TG], F32) for _ in range(2)]
    sg = [k.sb([128, TG], F32) for _ in range(2)]
    u1 = [k.sb([128, TG], F32) for _ in range(2)]
    gs = [k.sb([128, TG], F32) for _ in range(2)]
    bd_sb = [k.sb([1, D], BF16) for _ in range(2)]
    lgT_sb = k.sb([32, TG], F32)
    lg = k.sb([128, 4, 32], F32)
    ex = k.sb([128, 4, 32], F32)
    msk = k.sb([128, 4, 32], F32)
    m8 = k.sb([128, 4, 8], F32)
    negm = k.sb([128, 4], F32)
    ssum = k.sb([128, 4], F32)
    cc = k.sb([128, 4, 32], F32)
    P4 = [k.ps() for _ in range(4)]
    Yb = [k.ps() for _ in range(2)]
    ssq = k.ps()
    misc = k.ps()
    nslot = [0]

    def load_slot(src):
        sl = slots[nslot[0] % 3]
        nslot[0] += 1
        k.dma('sp', sl, src.rearrange("p (c n) -> p c n", n=512))
        return sl

    xTv = xT.rearrange("(c p) t -> p c t", p=128)
    mTv = mixT.rearrange("(c p) t -> p c t", p=128)
    x1v = x1T.rearrange("(c p) t -> p c t", p=128)
    for g in range(ng):
        tsl = slice(g * TG, (g + 1) * TG)
        k.dma('sp', xg, xTv[:, :, tsl])
        k.dma('sp', mg, mTv[:, :, tsl])
        for q in range(4):
            sl = load_slot(wout_bf[q])
            for dd in range(4):
                dch = q * 4 + dd
                pb = P4[dch % 4]
                for c in range(NCH):
                    k.mm(pb, sl[:, c, dd * 128:(dd + 1) * 128], mg[:, c, :], start=(c == 0), stop=(c == NCH - 1))
                k.stt('dve', xg[:, dch, :], pb, ga[:, dch:dch + 1], xg[:, dch, :], ALU.mult, ALU.add)
        if g < nown:
            k.dma('sp', x1v[:, :, tsl], xg)
        for c in range(NCH):
            k.act(sqb[c % 2], xg[:, c, :], AF.Square)
            k.mm(ssq, ones, sqb[c % 2], start=(c == 0), stop=(c == NCH - 1))
        k.act(rstd, ssq, AF.Sqrt, bias=EPS, scale=1.0 / D)
        k.recip(rstd, rstd)
        lgT = misc[0:32, :]
        for c in range(NCH):
            tb = tmpb[c % 2]
            hb = h32[c % 2]
            k.stt('dve', tb, xg[:, c, :], wf[:, c:c + 1], rstd, ALU.mult, ALU.mult)
            k.act(hb, tb, AF.Identity, bias=shf[:, c:c + 1])
            k.copy('pool', hT[:, c, :], hb)
            k.mm(lgT, wr[:, c, :], hb, start=(c == 0), stop=(c == NCH - 1))
        k.copy('dve', lgT_sb, lgT)
        for t in range(4):
            lp = misc[:, t * 32:(t + 1) * 32]
            k.mm(lp, lgT_sb[0:32, t * 128:(t + 1) * 128], ident[0:32, :])
        for t in range(4):
            lp = misc[:, t * 32:(t + 1) * 32]
            k.tt('dve', lg[:, t, :], lp, rb, ALU.add)
            k.P.add('dve', (lambda t: (lambda e: e.max(m8[:, t, :], lg[:, t, :])))(t), reads=[lg[:, t, :]], writes=[m8[:, t, :]])
            k.ts('dve', negm[:, t:t + 1], m8[:, t, 0:1], -1.0, None, ALU.mult)
            k.act(ex[:, t, :], lg[:, t, :], AF.Exp, bias=negm[:, t:t + 1])
            k.ts('dve', msk[:, t, :], lg[:, t, :], m8[:, t, 3:4], None, ALU.is_ge)
            k.tt('dve', ex[:, t, :], ex[:, t, :], msk[:, t, :], ALU.mult)
            k.P.add('dve', (lambda t: (lambda e: e.reduce_sum(ssum[:, t:t + 1], ex[:, t, :], AX.X)))(t),
                    reads=[ex[:, t, :]], writes=[ssum[:, t:t + 1]])
            k.recip(ssum[:, t:t + 1], ssum[:, t:t + 1])
            k.ts('dve', cc[:, t, :], ex[:, t, :], ssum[:, t:t + 1], None, ALU.mult)
        for e in range(NEL):
            bdb = bd_sb[e % 2]
            k.dma('pool', bdb, bd_d[0:1, e * D:(e + 1) * D])
            for s in range(8):
                sl = load_slot(wgu_bf[e][s])
                for pair in range(2):
                    j = 2 * s + pair
                    pp = j % 2
                    G = P4[2 * pp]
                    U = P4[2 * pp + 1]
                    for c in range(NCH):
                        k.mm(G, sl[:, c, pair * 128:(pair + 1) * 128], hT[:, c, :], start=(c == 0), stop=(c == NCH - 1))
                    for c in range(NCH):
                        k.mm(U, sl[:, c, 256 + pair * 128:256 + (pair + 1) * 128], hT[:, c, :], start=(c == 0), stop=(c == NCH - 1))
                    k.ts('dve', g1[pp], G, bgu[:, e * 32 + j:e * 32 + j + 1], 7.0, ALU.add, ALU.min)
                    k.act(sg[pp], g1[pp], AF.Sigmoid, scale=1.702)
                    k.ts('dve', u1[pp], U, bgu[:, e * 32 + 16 + j:e * 32 + 16 + j + 1], 7.0, ALU.add, ALU.min)
                    k.tt('pool', gs[pp], g1[pp], sg[pp], ALU.mult)
                    k.ts('dve', u1[pp], u1[pp], -7.0, 1.0, ALU.max, ALU.add)
                    k.tt('dve', actT[:, j, :], u1[pp], gs[pp], ALU.mult)
            for q in range(4):
                sl = load_slot(wd_bf[e][q])
                for t in range(4):
                    Y = Yb[(q * 4 + t) % 2]
                    for j in range(NCH):
                        k.mm(Y, actT[:, j, t * 128:(t + 1) * 128], sl[:, j, :], start=(j == 0), stop=False)
                    k.mm(Y, ones[0:1, 0:128], bdb[0:1, q * 512:(q + 1) * 512], start=False, stop=True)
                    ysl = yacc[:, t, q * 512:(q + 1) * 512]
                    if e == 0:
                        k.ts('dve', ysl, Y, cc[:, t, e:e + 1], None, ALU.mult)
                    else:
                        k.stt('dve', ysl, Y, cc[:, t, e:e + 1], ysl, ALU.mult, ALU.add)
        k.dma('sp', y[tsl, :].rearrange("(t p) d -> p t d", p=128), yacc)
    k.finish()
    return nc


def cd_inputs(inp, l, xT, mixT, mod_l, c, ng=NG):
    St = ng * TG
    sh = (c * 1024) % St
    perm = [(4 * c + i) % 32 for i in range(32)]
    ga, shf, scf = mod_l[2 * D:3 * D], mod_l[3 * D:4 * D], mod_l[4 * D:5 * D]
    vecs = np.concatenate([col16(ga), col16(scf), col16(shf), col16(inp['norm_ffn'][l])], axis=1)
    w_out = inp['w_out'][l]
    wout_t = w_out.reshape(NCH, 128, 4, 512).transpose(2, 1, 0, 3).reshape(4, 128, 8192)
    rw = inp['router_w'][l][:, perm]
    wr = rw.reshape(NCH, 128, 32).transpose(1, 0, 2).reshape(128, NCH * 32)
    rb = np.broadcast_to(inp['router_b'][l][perm][None, :], (128, 32))
    ex = perm[:NEL]
    wgu = inp['w_gate_up'][l][ex]
    wg = wgu[:, :, 0::2].reshape(NEL, NCH, 128, 8, 256)
    wu = wgu[:, :, 1::2].reshape(NEL, NCH, 128, 8, 256)
    wgu_t = np.concatenate([wg, wu], axis=4).transpose(0, 3, 2, 1, 4).reshape(NEL, 8, 128, 8192)
    bgu_e = inp['b_gate_up'][l][ex]
    bg = bgu_e[:, 0::2].reshape(NEL, NCH, 128)
    bu = bgu_e[:, 1::2].reshape(NEL, NCH, 128)
    bgu = np.concatenate([bg, bu], axis=1).transpose(2, 0, 1).reshape(128, NEL * 32)
    wd = inp['w_down'][l][ex]
    wd_t = wd.reshape(NEL, NCH, 128, 4, 512).transpose(0, 3, 2, 1, 4).reshape(NEL, 4, 128, 8192)
    bd = inp['b_down'][l][ex].reshape(1, NEL * D)
    f = lambda a: np.ascontiguousarray(a, dtype=np.float32)
    return {
        "xT": np.ascontiguousarray(np.roll(xT[:, :St], -sh, axis=1)),
        "mixT": np.ascontiguousarray(np.roll(mixT[:, :St], -sh, axis=1)),
        "wout_t": f(wout_t), "vecs": f(vecs), "wr": f(wr), "rb": f(rb), "wgu_t": f(wgu_t), "bgu": f(bgu),
        "wd_t": f(wd_t), "bd": f(bd), "ones": np.ones((128, 128), NPBF), "ident": np.eye(32, dtype=np.float32),
    }


def run_CD(inp, l, xT, mixT, mod_l):
    nc = build_CD()
    in_maps = [cd_inputs(inp, l, xT, mixT, mod_l, c) for c in range(NCORES)]
    res = run_bass_kernel_spmd(nc, in_maps, core_ids=list(range(NCORES)))
    x1 = np.zeros((S, D), np.float32)
    yparts = []
    for c in range(NCORES):
        x1[c * 1024:(c + 1) * 1024] = res.results[c]["x1T"].T
        yparts.append(np.roll(res.results[c]["y"], c * 1024, axis=0))
    return x1, yparts


def build_E(final):
    nc = bass.Bass("TRN2", target_bir_lowering=False)
    k = K(nc)
    x1 = nc.dram_tensor("x1", [1024, D], F32, kind="ExternalInput").ap()
    yp = nc.dram_tensor("yp", [NCORES, 1024, D], F32, kind="ExternalInput").ap()
    gfb = nc.dram_tensor("gfb", [128, D], F32, kind="ExternalInput").ap()
    nfb = nc.dram_tensor("nfb", [128, D], F32, kind="ExternalInput").ap()
    out = nc.dram_tensor("out", [1024, D], F32, kind="ExternalOutput").ap()
    gf = k.sb([128, D], F32)
    nf = k.sb([128, D], F32)
    k.dma('sp', gf, gfb)
    k.dma('sp', nf, nfb)
    xt = [k.sb([128, D], F32) for _ in range(2)]
    yt = [k.sb([128, D], F32) for _ in range(4)]
    acc = [k.sb([128, D], F32) for _ in range(2)]
    junk = k.sb([128, D], F32)
    st = k.sb([128, 8], F32)
    ny = 0
    for t in range(8):
        rows = slice(t * 128, (t + 1) * 128)
        x_ = xt[t % 2]
        a_ = acc[t % 2]
        k.dma('sp', x_, x1[rows, :])
        for c in range(NCORES):
            yb = yt[ny % 4]
            ny += 1
            k.dma('sp', yb, yp[c, rows, :])
            if c == 0:
                k.copy('pool', a_, yb)
            else:
                k.tt('dve' if c % 2 else 'pool', a_, a_, yb, ALU.add)
        k.tt('dve', a_, a_, gf, ALU.mult)
        k.tt('dve', a_, a_, x_, ALU.add)
        if final:
            k.act(junk, a_, AF.Square, accum_out=st[:, t:t + 1])
            k.act(st[:, t:t + 1], st[:, t:t + 1], AF.Sqrt, bias=EPS, scale=1.0 / D)
            k.recip(st[:, t:t + 1], st[:, t:t + 1])
            k.stt('dve', a_, a_, st[:, t:t + 1], nf, ALU.mult, ALU.mult)
        k.dma('sp', out[rows, :], a_)
    k.finish()
    return nc


def run_E(inp, x1, yparts, gate_f, final):
    nc = build_E(final)
    gfb = np.ascontiguousarray(np.broadcast_to(gate_f[None, :], (128, D)).astype(np.float32))
    nfb = np.ascontiguousarray(np.broadcast_to(inp['norm_final'][None, :], (128, D)).astype(np.float32))
    in_maps = []
    for c in range(NCORES):
        rows = slice(c * 1024, (c + 1) * 1024)
        in_maps.append({"x1": np.ascontiguousarray(x1[rows]),
                        "yp": np.ascontiguousarray(np.stack([yparts[j][rows] for j in range(NCORES)])),
                        "gfb": gfb, "nfb": nfb})
    res = run_bass_kernel_spmd(nc, in_maps, core_ids=list(range(NCORES)))
    return np.concatenate([res.results[c]["out"] for c in range(NCORES)], axis=0)


def kernel(**inp):
    inp = {k_: np.asarray(v) for k_, v in inp.items()}
    mod = run_M(inp)
    x = np.ascontiguousarray(inp['x'][0], dtype=np.float32)
    for l in range(2):
        xT = np.ascontiguousarray(x.T)
        mixT = run_AB(inp, l, xT, mod[l])
        x1, yparts = run_CD(inp, l, xT, mixT, mod[l])
        x = run_E(inp, x1, yparts, mod[l][5 * D:6 * D], final=(l == 1))
    return x[None].astype(np.float32)
```

```python
import math
import numpy as np
import ml_dtypes
import concourse.bass as bass
import concourse.mybir as mybir
from concourse.bass_utils import run_bass_kernel_spmd

F32 = mybir.dt.float32
BF16 = mybir.dt.bfloat16
I32 = mybir.dt.int32
ALU = mybir.AluOpType
AF = mybir.ActivationFunctionType
AX = mybir.AxisListType
NPBF = ml_dtypes.bfloat16

NCORES = 8
S = 8192
D = 2048
NCH = 16
TG = 512
NG = S // TG
EPS = 1e-5
SAME_ENGINE_SYNC = True
SEM_LIMIT = 30000


def _region(ap):
    t = ap.tensor
    name = t.name
    pairs = ap.ap
    off = int(ap.offset)
    if type(t).__name__.startswith('DRam'):
        hi = off
        for st, cnt in pairs:
            hi += abs(st) * (cnt - 1)
        return (name, 0, 0, off, hi)
    row = 1
    for s in t.shape[1:]:
        row *= s
    p0 = off // row
    f0 = off % row
    p1 = p0
    f1 = f0
    for st, cnt in pairs:
        if st >= row and st % row == 0:
            p1 += (st // row) * (cnt - 1)
        else:
            f1 += abs(st) * (cnt - 1)
    return (name, p0, p1, f0, f1)


class Op:
    __slots__ = ('eng', 'fn', 'idx', 'deps', 'dma_waits', 'signal', 'dma_key', 'inc')

    def __init__(self, eng, fn):
        self.eng = eng
        self.fn = fn
        self.deps = {}
        self.dma_waits = {}
        self.signal = False
        self.dma_key = None
        self.inc = 16


class Prog:
    ENG = ('pe', 'act', 'dve', 'pool', 'sp')

    def __init__(self, nc):
        self.nc = nc
        self.ops = {e: [] for e in self.ENG}
        self.tiles = {}
        self.dma_cnt = {}
        self.dma_sem = {}
        self.nsem = 0

    def _newsem(self, nm):
        self.nsem += 1
        return self.nc.alloc_semaphore(f"s{self.nsem}_{nm}")

    def _dep(self, op, op2):
        if op2 is op:
            return
        if op2.dma_key is not None:
            k = op2.dma_key
            op.dma_waits[k] = max(op.dma_waits.get(k, 0), self.dma_cnt[k])
            return
        if op2.eng == op.eng and op.dma_key is None:
            if op.eng == 'pe' or not SAME_ENGINE_SYNC:
                return
        if op.deps.get(op2.eng, -1) < op2.idx:
            op.deps[op2.eng] = op2.idx

    def add(self, eng, fn, reads=(), writes=(), dma_key=None, inc=16):
        op = Op(eng, fn)
        op.idx = len(self.ops[eng])
        op.dma_key = dma_key
        op.inc = inc
        self.ops[eng].append(op)
        accs = [(_region(a), False) for a in reads] + [(_region(a), True) for a in writes]
        for (r, w) in accs:
            lst = self.tiles.get(r[0])
            if lst is None:
                continue
            psum = r[0].startswith('ps')
            for e in lst:
                if psum and e[4].eng != eng:
                    self._dep(op, e[4])
                elif (w or e[5]) and not (e[1] < r[1] or e[0] > r[2] or e[3] < r[3] or e[2] > r[4]):
                    self._dep(op, e[4])
        if dma_key is not None:
            self.dma_cnt[dma_key] = self.dma_cnt.get(dma_key, 0) + inc
        for (r, w) in accs:
            lst = self.tiles.setdefault(r[0], [])
            if w:
                lst[:] = [e for e in lst if not (e[0] >= r[1] and e[1] <= r[2] and e[2] >= r[3] and e[3] <= r[4])]
            else:
                lst[:] = [e for e in lst if not ((not e[5]) and e[4].eng == eng and e[4].dma_key is None
                                                 and dma_key is None
                                                 and e[0] >= r[1] and e[1] <= r[2] and e[2] >= r[3] and e[3] <= r[4])]
            lst.append([r[1], r[2], r[3], r[4], op, w])
        return op

    def dma(self, eng, out, in_, key=None, **kw):
        if key is None:
            od = type(out.tensor).__name__.startswith('DRam')
            idr = type(in_.tensor).__name__.startswith('DRam')
            key = out.tensor.name if (not od or idr) else in_.tensor.name
        return self.add(eng, lambda e: e.dma_start(out=out, in_=in_, **kw), reads=[in_], writes=[out], dma_key=key)

    def barrier(self):
        lasts = {}
        for e in self.ENG:
            j = len(self.ops[e]) - 1
            while j >= 0 and self.ops[e][j].dma_key is not None:
                j -= 1
            lasts[e] = j
        cnts = dict(self.dma_cnt)
        for e in self.ENG:
            op = Op(e, lambda eng: eng.nop())
            op.idx = len(self.ops[e])
            self.ops[e].append(op)
            for e2, j in lasts.items():
                if j >= 0 and e2 != e:
                    op.deps[e2] = j
            for k, c in cnts.items():
                op.dma_waits[k] = c
        self.tiles = {}

    def emit(self):
        nc = self.nc
        for e in self.ENG:
            for op in self.ops[e]:
                for e2, j in op.deps.items():
                    self.ops[e2][j].signal = True
        tick = {}
        for e in self.ENG:
            sem = None
            cnt = SEM_LIMIT + 1
            lst = []
            for op in self.ops[e]:
                if op.signal:
                    if cnt >= SEM_LIMIT:
                        sem = self._newsem(e)
                        cnt = 0
                    cnt += 1
                    lst.append((sem, cnt))
                else:
                    lst.append(None)
            tick[e] = lst
        for k in self.dma_cnt:
            self.dma_sem[k] = self._newsem('d')

        def run(e, eng):
            waited = {}
            for i, op in enumerate(self.ops[e]):
                for e2, j in op.deps.items():
                    sem, v = tick[e2][j]
                    key = id(sem)
                    if waited.get(key, 0) >= v:
                        continue
                    waited[key] = v
                    eng.wait_ge(sem, v)
                for k, c in op.dma_waits.items():
                    sem = self.dma_sem[k]
                    key = id(sem)
                    if waited.get(key, 0) >= c:
                        continue
                    waited[key] = c
                    eng.wait_ge(sem, c)
                inst = op.fn(eng)
                if op.dma_key is not None:
                    inst.then_inc(self.dma_sem[op.dma_key], op.inc)
                elif op.signal:
                    s, v = tick[e][i]
                    inst.then_inc(s, 1)

        with nc.Block() as block:
            @block.tensor
            def _(eng):
                run('pe', eng)

            @block.scalar
            def _(eng):
                run('act', eng)

            @block.vector
            def _(eng):
                run('dve', eng)

            @block.gpsimd
            def _(eng):
                run('pool', eng)

            @block.sync
            def _(eng):
                run('sp', eng)


class K:
    def __init__(self, nc):
        self.nc = nc
        self.P = Prog(nc)
        self.n = 0

    def sb(self, shape, dt, name=None):
        self.n += 1
        return self.nc.alloc_sbuf_tensor(name or f"sb{self.n}", list(shape), dt).ap()

    def ps(self, shape=(128, 512), dt=F32, name=None):
        self.n += 1
        return self.nc.alloc_psum_tensor(name or f"ps{self.n}", list(shape), dt).ap()

    def mm(self, out, lhsT, rhs, start=True, stop=True):
        self.P.add('pe', lambda e: e.matmul(out, lhsT, rhs, start=start, stop=stop), reads=[lhsT, rhs], writes=[out])

    def act(self, out, in_, func, bias=None, scale=1.0, accum_out=None):
        reads = [in_]
        kw = {}
        if bias is not None:
            kw['bias'] = bias
            if not isinstance(bias, (int, float)):
                reads.append(bias)
        if not isinstance(scale, (int, float)):
            reads.append(scale)
        writes = [out]
        if accum_out is not None:
            kw['accum_out'] = accum_out
            writes.append(accum_out)
        self.P.add('act', lambda e: e.activation(out=out, in_=in_, func=func, scale=scale, **kw), reads=reads, writes=writes)

    def ts(self, eng, out, in0, s1, s2, op0, op1=None):
        reads = [in0] + [s for s in (s1, s2) if s is not None and not isinstance(s, (int, float))]
        if op1 is None:
            self.P.add(eng, lambda e: e.tensor_scalar(out, in0, s1, None, op0), reads=reads, writes=[out])
        else:
            self.P.add(eng, lambda e: e.tensor_scalar(out, in0, s1, s2, op0, op1), reads=reads, writes=[out])

    def tt(self, eng, out, in0, in1, op):
        self.P.add(eng, lambda e: e.tensor_tensor(out, in0, in1, op), reads=[in0, in1], writes=[out])

    def stt(self, eng, out, in0, scalar, in1, op0, op1):
        reads = [in0, in1] + ([] if isinstance(scalar, (int, float)) else [scalar])
        self.P.add(eng, lambda e: e.scalar_tensor_tensor(out, in0, scalar, in1, op0, op1), reads=reads, writes=[out])

    def copy(self, eng, out, in_):
        self.P.add(eng, lambda e: e.tensor_copy(out, in_), reads=[in_], writes=[out])

    def recip(self, out, in_):
        self.P.add('dve', lambda e: e.reciprocal(out, in_), reads=[in_], writes=[out])

    def memset(self, eng, ap, v):
        self.P.add(eng, lambda e: e.memset(ap, v), writes=[ap])

    def dma(self, eng, out, in_, **kw):
        self.P.dma(eng, out, in_, **kw)

    def finish(self):
        self.P.barrier()
        self.P.emit()


def col16(v):
    return np.ascontiguousarray(np.asarray(v, np.float32).reshape(NCH, 128).T)


MCOL = 12288 // NCORES
MCH = MCOL // 128


def build_M():
    nc = bass.Bass("TRN2", target_bir_lowering=False)
    k = K(nc)
    cvec = nc.dram_tensor("cvec", [128, NCH], F32, kind="ExternalInput").ap()
    w = nc.dram_tensor("w", [2, D, MCOL], F32, kind="ExternalInput").ap()
    b = nc.dram_tensor("b", [128, 2 * MCH], F32, kind="ExternalInput").ap()
    out = nc.dram_tensor("out", [128, 2 * MCH], F32, kind="ExternalOutput").ap()
    c_sb = k.sb([128, NCH], F32)
    ca = k.sb([128, NCH], F32)
    b_sb = k.sb([128, 2 * MCH], F32)
    o_sb = k.sb([128, 2 * MCH], F32)
    w_sb = [k.sb([128, NCH, MCOL // 2], F32) for _ in range(2)]
    ps = k.ps([128, 2 * MCH])
    k.dma('sp', c_sb, cvec)
    k.dma('sp', b_sb, b)
    k.act(ca, c_sb, AF.Silu)
    for l in range(2):
        for hf in range(2):
            wt = w_sb[hf]
            k.dma('sp', wt, w[l].rearrange("(c p) n -> p c n", p=128)[:, :, hf * (MCOL // 2):(hf + 1) * (MCOL // 2)])
            for jj in range(MCH // 2):
                j = l * MCH + hf * (MCH // 2) + jj
                for c in range(NCH):
                    k.mm(ps[:, j:j + 1], wt[:, c, jj * 128:(jj + 1) * 128], ca[:, c:c + 1], start=(c == 0), stop=(c == NCH - 1))
    k.tt('dve', o_sb, ps, b_sb, ALU.add)
    k.dma('sp', out, o_sb)
    k.finish()
    return nc


def run_M(inp):
    nc = build_M()
    in_maps = []
    for c in range(NCORES):
        sl = slice(c * MCOL, (c + 1) * MCOL)
        bb = np.concatenate([inp['ada_b'][l][sl].reshape(MCH, 128).T for l in range(2)], axis=1)
        in_maps.append({
            "cvec": col16(inp['c'][0]),
            "w": np.ascontiguousarray(inp['ada_w'][:, :, sl]),
            "b": np.ascontiguousarray(bb.astype(np.float32)),
        })
    res = run_bass_kernel_spmd(nc, in_maps, core_ids=list(range(NCORES)))
    mod = np.zeros((2, 12288), np.float32)
    for c in range(NCORES):
        o = res.results[c]["out"]
        for l in range(2):
            mod[l, c * MCOL:(c + 1) * MCOL] = o[:, l * MCH:(l + 1) * MCH].T.reshape(-1)
    return mod


def ab_consts():
    c = {}
    p = np.arange(128)
    inv = (10000.0 ** (-(np.arange(32, dtype=np.float32)) / 32)).astype(np.float32)
    c['invf'] = (inv[p % 32].astype(np.float64) / (2 * np.pi)).astype(np.float32).reshape(128, 1)
    rot = np.zeros((128, 128), np.float32)
    for m in range(128):
        if (m % 64) < 32:
            rot[m + 32, m] = -1.0
        else:
            rot[m - 32, m] = 1.0
    c['rot'] = rot.astype(NPBF)
    c['ones'] = np.ones((128, 128), NPBF)
    kk = np.arange(128)[:, None]
    qq = np.arange(512)[None, :]
    c['maskd'] = np.stack([((128 * j + kk) <= qq) for j in range(4)], 1).astype(NPBF)
    c['masks'] = np.stack([((128 * j + kk) < qq) for j in range(4)], 1).astype(NPBF)
    jj = np.arange(128)[:, None]
    ss = np.arange(128)[None, :]
    c['negtri'] = (-(jj >= ss).astype(np.float32)).astype(NPBF)
    c['negones'] = (-np.ones((1, 128), np.float32)).astype(NPBF)
    return c


def build_AB(lam_init, ng=NG, stop=None, cast_w=False):
    nc = bass.Bass("TRN2", target_bir_lowering=False)
    k = K(nc)
    St = ng * TG
    nkb = St // 128
    xT = nc.dram_tensor("xT", [D, St], F32, kind="ExternalInput").ap()
    posb = nc.dram_tensor("posb", [128, St], I32, kind="ExternalInput").ap()
    win = nc.dram_tensor("win", [D, 768], F32, kind="ExternalInput").ap()
    vecs = nc.dram_tensor("vecs", [128, 3 * NCH], F32, kind="ExternalInput").ap()
    lamv = nc.dram_tensor("lamv", [128, 4 * 64], F32, kind="ExternalInput").ap()
    hw = nc.dram_tensor("hw", [128, 2], F32, kind="ExternalInput").ap()
    invf_d = nc.dram_tensor("invf", [128, 1], F32, kind="ExternalInput").ap()
    rot_d = nc.dram_tensor("rot", [128, 128], BF16, kind="ExternalInput").ap()
    ones_d = nc.dram_tensor("ones", [128, 128], BF16, kind="ExternalInput").ap()
    maskd_d = nc.dram_tensor("maskd", [128, 4, 512], BF16, kind="ExternalInput").ap()
    masks_d = nc.dram_tensor("masks", [128, 4, 512], BF16, kind="ExternalInput").ap()
    negtri_d = nc.dram_tensor("negtri", [128, 128], BF16, kind="ExternalInput").ap()
    negones_d = nc.dram_tensor("negones", [1, 128], BF16, kind="ExternalInput").ap()
    mixd = nc.dram_tensor("mixd", [128, St], BF16, kind="ExternalOutput").ap()
    mixs = nc.dram_tensor("mixs", [128, St], BF16, kind="ExternalOutput").ap()
    if cast_w:
        wgu_t = nc.dram_tensor("wgu_t", [NEL * 8, 128, 8192], F32, kind="ExternalInput").ap()
        wd_t = nc.dram_tensor("wd_t", [NEL * 4, 128, 8192], F32, kind="ExternalInput").ap()
        wgu_bf = nc.dram_tensor("wgu_bf", [NEL * 8, 128, 8192], BF16, kind="ExternalOutput").ap()
        wd_bf = nc.dram_tensor("wd_bf", [NEL * 4, 128, 8192], BF16, kind="ExternalOutput").ap()

    win_sb = k.sb([128, NCH, 768], BF16)
    vec_sb = k.sb([128, 3 * NCH], F32)
    wa = k.sb([128, NCH], F32)
    lam_sb = k.sb([128, 256], F32)
    hw_sb = k.sb([128, 2], F32)
    invf = k.sb([128, 1], F32)
    rot = k.sb([128, 128], BF16)
    ones = k.sb([128, 128], BF16)
    maskd = k.sb([128, 4, 512], BF16)
    masks = k.sb([128, 4, 512], BF16)
    negtri = k.sb([128, 128], BF16)
    negones = k.sb([1, 128], BF16)
    qd = k.sb([128, St], BF16)
    kd = k.sb([128, St], BF16)
    qs = k.sb([128, St], BF16)
    ks = k.sb([128, St], BF16)
    vd = k.sb([128, nkb, 128], BF16)
    vs = k.sb([128, nkb, 128], BF16)
    neglam = k.sb([128, 1], F32)
    wsub = k.sb([128, 1], F32)

    k.dma('pool', win_sb, win.rearrange("(c p) n -> p c n", p=128))
    if cast_w:
        for e in range(NEL):
            for s_ in range(8):
                k.dma('pool', wgu_bf[e * 8 + s_].rearrange("p (a b) -> p a b", b=2048),
                      wgu_t[e * 8 + s_].rearrange("p (a b) -> p a b", b=2048))
            for q in range(4):
                k.dma('pool', wd_bf[e * 4 + q].rearrange("p (a b) -> p a b", b=2048),
                      wd_t[e * 4 + q].rearrange("p (a b) -> p a b", b=2048))
    for dst, src in ((vec_sb, vecs), (lam_sb, lamv), (hw_sb, hw), (invf, invf_d), (rot, rot_d), (ones, ones_d),
                     (maskd, maskd_d), (masks, masks_d), (negtri, negtri_d), (negones, negones_d)):
        k.dma('sp', dst, src)
    k.stt('dve', wa, vec_sb[:, NCH:2 * NCH], 1.0, vec_sb[:, 2 * NCH:3 * NCH], ALU.add, ALU.mult)
    sha = vec_sb[:, 0:NCH]
    lt = k.sb([128, 128], F32)
    l12 = k.sb([128, 2], F32)
    k.tt('dve', lt[:, 0:64], lam_sb[:, 0:64], lam_sb[:, 64:128], ALU.mult)
    k.tt('dve', lt[:, 64:128], lam_sb[:, 128:192], lam_sb[:, 192:256], ALU.mult)
    k.P.add('dve', lambda e: e.reduce_sum(l12[:, 0:1], lt[:, 0:64], AX.X), reads=[lt[:, 0:64]], writes=[l12[:, 0:1]])
    k.P.add('dve', lambda e: e.reduce_sum(l12[:, 1:2], lt[:, 64:128], AX.X), reads=[lt[:, 64:128]], writes=[l12[:, 1:2]])
    k.act(l12, l12, AF.Exp)
    k.tt('dve', neglam, l12[:, 1:2], l12[:, 0:1], ALU.subtract)
    k.ts('dve', neglam, neglam, -float(lam_init), None, ALU.add)
    k.ts('dve', wsub, hw_sb[:, 0:1], float(1.0 - lam_init), None, ALU.mult)
    sbw = hw_sb[:, 1:2]

    if stop == 'S':
        k.finish()
        return nc
    xg = k.sb([128, NCH, TG], F32)
    hT = k.sb([128, NCH, TG], BF16)
    sqb = [k.sb([128, TG], BF16) for _ in range(2)]
    tmpb = [k.sb([128, TG], F32) for _ in range(2)]
    rstd = k.sb([128, TG], F32)
    posi = k.sb([128, TG], I32)
    posf = k.sb([128, TG], F32)
    uc = k.sb([128, TG], F32)
    rr = k.sb([128, TG], F32)
    cosT = k.sb([128, TG], F32)
    sinT = k.sb([128, TG], F32)
    xsb = [k.sb([128, TG], BF16) for _ in range(2)]
    t1 = k.sb([128, TG], F32)
    t2 = k.sb([128, TG], F32)
    pbank = [k.ps() for _ in range(8)]

    for g in range(ng):
        tsl = slice(g * TG, (g + 1) * TG)
        k.dma('sp', xg, xT.rearrange("(c p) t -> p c t", p=128)[:, :, tsl])
        k.dma('sp', posi, posb[:, tsl])
        ssq = pbank[0]
        for c in range(NCH):
            k.act(sqb[c % 2], xg[:, c, :], AF.Square)
            k.mm(ssq, ones, sqb[c % 2], start=(c == 0), stop=(c == NCH - 1))
        k.act(rstd, ssq, AF.Sqrt, bias=EPS, scale=1.0 / D)
        k.recip(rstd, rstd)
        for c in range(NCH):
            tb = tmpb[c % 2]
            k.stt('dve', tb, xg[:, c, :], wa[:, c:c + 1], rstd, ALU.mult, ALU.mult)
            k.act(hT[:, c, :], tb, AF.Identity, bias=sha[:, c:c + 1])
        k.copy('dve', posf, posi)
        k.ts('dve', uc, posf, invf[:, 0:1], None, ALU.mult)
        for (tab, shift) in ((sinT, 0.0), (cosT, 0.25)):
            if shift:
                k.ts('dve', rr, uc, shift, None, ALU.add)
                src = rr
            else:
                src = uc
            k.ts('dve', t1, src, 12582912.0, 12582912.0, ALU.add, ALU.subtract)
            k.tt('dve', t2, src, t1, ALU.subtract)
            k.act(tab, t2, AF.Sin, scale=2.0 * math.pi)
        for o in range(4):
            pb = pbank[1 + o]
            for c in range(NCH):
                k.mm(pb, win_sb[:, c, o * 128:(o + 1) * 128], hT[:, c, :], start=(c == 0), stop=(c == NCH - 1))
        for t in range(TG // 128):
            pv = pbank[5 + (t % 2)]
            for c in range(NCH):
                k.mm(pv[:, 0:256], hT[:, c, t * 128:(t + 1) * 128], win_sb[:, c, 512:768], start=(c == 0), stop=(c == NCH - 1))
            kb = g * (TG // 128) + t
            k.copy('dve', vd[:, kb, :], pv[:, 0:128])
            k.act(vs[:, kb, :], pv[:, 128:256], AF.Copy)
        k.act(qs[:, tsl], pbank[3], AF.Copy, scale=float(128 ** -0.5))
        k.act(ks[:, tsl], pbank[4], AF.Copy)
        for i, (pb, dst, sc) in enumerate(((pbank[1], qd, 0.125), (pbank[2], kd, 1.0))):
            k.act(xsb[i], pb, AF.Copy)
            rp = pbank[7]
            k.mm(rp, rot, xsb[i])
            k.tt('dve', t1, xsb[i], cosT, ALU.mult)
            k.tt('dve', t2, rp, sinT, ALU.mult)
            k.stt('dve', dst[:, tsl], t1, sc, t2, ALU.mult, ALU.add) if sc == 1.0 else \
                k.stt('dve', t1, t1, 1.0, t2, ALU.mult, ALU.add)
            if sc != 1.0:
                k.ts('dve', dst[:, tsl], t1, sc, None, ALU.mult)

    if stop == 'A':
        k.finish()
        return nc
    pT = [[hT[:, 0, :], hT[:, 1, :]], [hT[:, 2, :], hT[:, 3, :]]]
    ez = [sinT, t1]
    sp = [hT[:, 4, :], hT[:, 5, :]]
    aT = [hT[:, 6, :], hT[:, 7, :]]
    Rsb = [hT[0:1, 8, :], hT[0:1, 9, :]]
    o1 = rstd
    o2 = posf
    rd = uc
    af = rr
    sqa = hT[:, 10, :]
    rs2 = cosT
    outb = [hT[:, 11, :], hT[:, 12, :]]

    def head_norm_out(src_f32, wcol, dst_dram, tsl, bank, ob):
        k.act(sqa, src_f32, AF.Square)
        k.mm(bank, ones, sqa)
        k.act(rs2, bank, AF.Sqrt, bias=EPS, scale=1.0 / 128)
        k.recip(rs2, rs2)
        k.stt('dve', ob, src_f32, wcol, rs2, ALU.mult, ALU.mult)
        k.dma('sp', dst_dram[:, tsl], ob)

    A1 = [pbank[0], pbank[1]]
    A2 = [pbank[2], pbank[3]]
    O1, D1, O2, D2 = pbank[4], pbank[5], pbank[6], pbank[7]
    for G in range(ng):
        tsl = slice(G * TG, (G + 1) * TG)
        nb = 4 * (G + 1)

        def st1(b):
            par = b % 2
            ksl = slice(b * 128, (b + 1) * 128)
            k.mm(A1[par], kd[0:64, ksl], qd[0:64, tsl])
            k.mm(A2[par], kd[64:128, ksl], qd[64:128, tsl])
            k.act(pT[par][0], A1[par], AF.Exp)
            k.act(pT[par][1], A2[par], AF.Exp)
            jj = b - 4 * G
            if jj >= 0:
                k.tt('pool', pT[par][0], pT[par][0], maskd[:, jj, :], ALU.mult)
                k.tt('pool', pT[par][1], pT[par][1], maskd[:, jj, :], ALU.mult)

        def st2(b):
            par = b % 2
            first = (b == 0)
            last = (b == nb - 1)
            k.mm(O1, vd[:, b, :], pT[par][0], start=first, stop=last)
            k.mm(D1, ones, pT[par][0], start=first, stop=last)
            k.mm(O2, vd[:, b, :], pT[par][1], start=first, stop=last)
            k.mm(D2, ones, pT[par][1], start=first, stop=last)

        for i in range(nb + 1):
            if i < nb:
                st1(i)
            if i >= 1:
                st2(i - 1)
        k.recip(rd, D1)
        k.tt('dve', o1, O1, rd, ALU.mult)
        k.recip(rd, D2)
        k.tt('dve', o2, O2, rd, ALU.mult)
        k.stt('dve', af, o2, neglam[:, 0:1], o1, ALU.mult, ALU.add)
        head_norm_out(af, wsub[:, 0:1], mixd, tsl, A1[0], outb[G % 2])

    if stop == 'B1':
        k.finish()
        return nc
    Az = [pbank[0], pbank[1]]
    Bz = [pbank[2], pbank[3]]
    Rn = [pbank[4], pbank[5]]
    Os = pbank[6]
    ssb = pbank[7]
    for G in range(ng):
        tsl = slice(G * TG, (G + 1) * TG)
        nb = 4 * (G + 1)
        order = list(range(nb - 1, -1, -1))

        def s1(i):
            b = order[i]
            par = i % 2
            ksl = slice(b * 128, (b + 1) * 128)
            k.mm(Az[par], ks[:, ksl], qs[:, tsl])
            k.act(ez[par], Az[par], AF.Exp)
            k.act(sp[par], ez[par], AF.Ln, bias=1.0)
            jj = b - 4 * G
            if jj >= 0:
                k.tt('pool', sp[par], sp[par], masks[:, jj, :], ALU.mult)

        def s2(i):
            b = order[i]
            par = i % 2
            ksl = slice(b * 128, (b + 1) * 128)
            first = (i == 0)
            k.mm(Bz[par], ks[:, ksl], qs[:, tsl], start=True, stop=False)
            k.mm(Bz[par], negtri, sp[par], start=False, stop=first)
            if not first:
                k.mm(Bz[par], negones[0:1, :], Rsb[(i - 1) % 2][0:1, :], start=False, stop=True)
            if i < nb - 1:
                k.mm(Rn[par][0:1, :], ones[:, 0:1], sp[par], start=True, stop=first)
                if not first:
                    k.mm(Rn[par][0:1, :], ones[0:1, 0:1], Rsb[(i - 1) % 2][0:1, :], start=False, stop=True)
                k.copy('dve', Rsb[i % 2][0:1, :], Rn[par][0:1, :])
            k.act(aT[par], Bz[par], AF.Exp)
            jj = b - 4 * G
            if jj >= 0:
                k.tt('pool', aT[par], aT[par], masks[:, jj, :], ALU.mult)
            k.mm(Os, vs[:, b, :], aT[par], start=first, stop=(i == nb - 1))

        for i in range(nb + 1):
            if i < nb:
                s1(i)
            if i >= 1:
                s2(i - 1)
        k.act(af, Os, AF.Copy)
        head_norm_out(af, sbw, mixs, tsl, ssb, outb[G % 2])
    k.finish()
    return nc


def expert_tiles(inp, l, c):
    ex = [4 * c + i for i in range(NEL)]
    wgu = inp['w_gate_up'][l][ex]
    wg = wgu[:, :, 0::2].reshape(NEL, NCH, 128, 8, 256)
    wu = wgu[:, :, 1::2].reshape(NEL, NCH, 128, 8, 256)
    wgu_t = np.concatenate([wg, wu], axis=4).transpose(0, 3, 2, 1, 4).reshape(NEL * 8, 128, 8192)
    wd = inp['w_down'][l][ex]
    wd_t = wd.reshape(NEL, NCH, 128, 4, 512).transpose(0, 3, 2, 1, 4).reshape(NEL * 4, 128, 8192)
    return np.ascontiguousarray(wgu_t, dtype=np.float32), np.ascontiguousarray(wd_t, dtype=np.float32)


def lam_init_fn(l):
    return 0.8 - 0.6 * math.exp(-0.3 * l)


def run_AB(inp, l, xT, mod_l, ng=NG, stop=None, cast_w=False):
    St = ng * TG
    nc = build_AB(lam_init_fn(l), ng=ng, stop=stop, cast_w=cast_w)
    cst = ab_consts()
    sha, sca = mod_l[0:D], mod_l[D:2 * D]
    vecs = np.concatenate([col16(sha), col16(sca), col16(inp['norm_mix'][l])], axis=1)
    lamv = np.concatenate([inp['lambda_q1'][l], inp['lambda_k1'][l], inp['lambda_q2'][l], inp['lambda_k2'][l]])
    lamv = np.ascontiguousarray(np.broadcast_to(lamv[None, :], (128, 256)).astype(np.float32))
    hw = np.ascontiguousarray(np.stack([inp['subln_w'][l], inp['sb_norm_w'][l]], axis=1).astype(np.float32))
    posb = np.ascontiguousarray(np.broadcast_to(inp['positions'][0][None, :St], (128, St)).astype(np.int32))
    w_in = inp['w_in'][l]
    xTs = np.ascontiguousarray(xT[:, :St])
    in_maps = []
    for c in range(NCORES):
        cols = []
        for base in (0, 1024, 3072, 4096, 2048, 5120):
            cols.append(w_in[:, base + c * 128: base + (c + 1) * 128])
        m = {"xT": xTs, "posb": posb, "win": np.ascontiguousarray(np.concatenate(cols, axis=1)),
             "vecs": np.ascontiguousarray(vecs), "lamv": lamv, "hw": hw}
        m.update(cst)
        if cast_w:
            wgu_t, wd_t = expert_tiles(inp, l, c)
            m["wgu_t"] = wgu_t
            m["wd_t"] = wd_t
        in_maps.append(m)
    res = run_bass_kernel_spmd(nc, in_maps, core_ids=list(range(NCORES)))
    del in_maps
    mixT = np.zeros((D, St), NPBF)
    for c in range(NCORES):
        mixT[c * 128:(c + 1) * 128] = res.results[c]["mixd"]
        mixT[1024 + c * 128:1024 + (c + 1) * 128] = res.results[c]["mixs"]
    if cast_w:
        return mixT, [(res.results[c]["wgu_bf"], res.results[c]["wd_bf"]) for c in range(NCORES)]
    return mixT


NEL = 4
CTOK = 1024


def build_C(ng=CTOK // TG):
    nc = bass.Bass("TRN2", target_bir_lowering=False)
    k = K(nc)
    St = ng * TG
    xT = nc.dram_tensor("xT", [D, St], F32, kind="ExternalInput").ap()
    mixT = nc.dram_tensor("mixT", [D, St], BF16, kind="ExternalInput").ap()
    wout_t = nc.dram_tensor("wout_t", [4, 128, 8192], F32, kind="ExternalInput").ap()
    vecs = nc.dram_tensor("vecs", [128, 4 * NCH], F32, kind="ExternalInput").ap()
    wr_d = nc.dram_tensor("wr", [128, NCH * 32], F32, kind="ExternalInput").ap()
    rb_d = nc.dram_tensor("rb", [128, 32], F32, kind="ExternalInput").ap()
    ones_d = nc.dram_tensor("ones", [128, 128], BF16, kind="ExternalInput").ap()
    id_d = nc.dram_tensor("ident", [32, 32], F32, kind="ExternalInput").ap()
    x1T = nc.dram_tensor("x1T", [D, St], F32, kind="ExternalOutput").ap()
    hTo = nc.dram_tensor("hT", [D, St], BF16, kind="ExternalOutput").ap()
    cco = nc.dram_tensor("cc", [St, 32], F32, kind="ExternalOutput").ap()

    wout_sb = [k.sb([128, NCH, 512], BF16) for _ in range(4)]
    for q in range(4):
        k.dma('pool', wout_sb[q].rearrange("p c (a b) -> p (c a) b", b=512).rearrange("p (x y) b -> p x (y b)", y=4),
              wout_t[q].rearrange("p (x z) -> p x z", z=2048))
    vec_sb = k.sb([128, 4 * NCH], F32)
    wf = k.sb([128, NCH], F32)
    wr = k.sb([128, NCH, 32], F32)
    rb = k.sb([128, 32], F32)
    ones = k.sb([128, 128], BF16)
    ident = k.sb([32, 32], F32)
    k.dma('sp', vec_sb, vecs)
    k.dma('sp', wr, wr_d.rearrange("p (c e) -> p c e", e=32))
    k.dma('sp', rb, rb_d)
    k.dma('sp', ones, ones_d)
    k.dma('sp', ident, id_d)
    ga = vec_sb[:, 0:NCH]
    shf = vec_sb[:, 2 * NCH:3 * NCH]
    k.stt('dve', wf, vec_sb[:, NCH:2 * NCH], 1.0, vec_sb[:, 3 * NCH:4 * NCH], ALU.add, ALU.mult)

    xgs = [k.sb([128, NCH, TG], F32)] * 2
    mgs = [k.sb([128, NCH, TG], BF16) for _ in range(2)]
    hTs = [k.sb([128, NCH, TG], BF16)] * 2
    sqb = [k.sb([128, TG], BF16) for _ in range(2)]
    tmpb = [k.sb([128, TG], F32) for _ in range(2)]
    h32 = [k.sb([128, TG], F32) for _ in range(2)]
    rstd = k.sb([128, TG], F32)
    lgT_sb = k.sb([32, TG], F32)
    lg = k.sb([128, 4, 32], F32)
    ex = k.sb([128, 4, 32], F32)
    msk = k.sb([128, 4, 32], F32)
    m8 = k.sb([128, 4, 8], F32)
    negm = k.sb([128, 4], F32)
    ssum = k.sb([128, 4], F32)
    ccs = [k.sb([128, 4, 32], F32) for _ in range(2)]
    P4 = [k.ps() for _ in range(4)]
    ssq = k.ps()
    misc = k.ps()

    xTv = xT.rearrange("(c p) t -> p c t", p=128)
    mTv = mixT.rearrange("(c p) t -> p c t", p=128)
    x1v = x1T.rearrange("(c p) t -> p c t", p=128)
    hTv = hTo.rearrange("(c p) t -> p c t", p=128)
    for g in range(ng):
        tsl = slice(g * TG, (g + 1) * TG)
        xg, mg, hT, cc = xgs[g % 2], mgs[g % 2], hTs[g % 2], ccs[g % 2]
        k.dma('sp', xg, xTv[:, :, tsl])
        k.dma('sp', mg, mTv[:, :, tsl])
        for q in range(4):
            sl = wout_sb[q]
            for dd in range(4):
                dch = q * 4 + dd
                pb = P4[dch % 4]
                for c in range(NCH):
                    k.mm(pb, sl[:, c, dd * 128:(dd + 1) * 128], mg[:, c, :], start=(c == 0), stop=(c == NCH - 1))
                k.stt('dve', xg[:, dch, :], pb, ga[:, dch:dch + 1], xg[:, dch, :], ALU.mult, ALU.add)
        k.dma('sp', x1v[:, :, tsl], xg)
        for c in range(NCH):
            k.act(sqb[c % 2], xg[:, c, :], AF.Square)
            k.mm(ssq, ones, sqb[c % 2], start=(c == 0), stop=(c == NCH - 1))
        k.act(rstd, ssq, AF.Sqrt, bias=EPS, scale=1.0 / D)
        k.recip(rstd, rstd)
        lgT = misc[0:32, :]
        for c in range(NCH):
            tb = tmpb[c % 2]
            hb = h32[c % 2]
            k.stt('dve', tb, xg[:, c, :], wf[:, c:c + 1], rstd, ALU.mult, ALU.mult)
            k.act(hb, tb, AF.Identity, bias=shf[:, c:c + 1])
            k.copy('pool', hT[:, c, :], hb)
            k.mm(lgT, wr[:, c, :], hb, start=(c == 0), stop=(c == NCH - 1))
        k.dma('sp', hTv[:, :, tsl], hT)
        k.copy('dve', lgT_sb, lgT)
        for t in range(4):
            lp = misc[:, t * 32:(t + 1) * 32]
            k.mm(lp, lgT_sb[0:32, t * 128:(t + 1) * 128], ident[0:32, :])
        for t in range(4):
            lp = misc[:, t * 32:(t + 1) * 32]
            k.tt('dve', lg[:, t, :], lp, rb, ALU.add)
            k.P.add('dve', (lambda t: (lambda e: e.max(m8[:, t, :], lg[:, t, :])))(t), reads=[lg[:, t, :]], writes=[m8[:, t, :]])
            k.ts('dve', negm[:, t:t + 1], m8[:, t, 0:1], -1.0, None, ALU.mult)
            k.act(ex[:, t, :], lg[:, t, :], AF.Exp, bias=negm[:, t:t + 1])
            k.ts('dve', msk[:, t, :], lg[:, t, :], m8[:, t, 3:4], None, ALU.is_ge)
            k.tt('dve', ex[:, t, :], ex[:, t, :], msk[:, t, :], ALU.mult)
            k.P.add('dve', (lambda t: (lambda e: e.reduce_sum(ssum[:, t:t + 1], ex[:, t, :], AX.X)))(t),
                    reads=[ex[:, t, :]], writes=[ssum[:, t:t + 1]])
            k.recip(ssum[:, t:t + 1], ssum[:, t:t + 1])
            k.ts('dve', cc[:, t, :], ex[:, t, :], ssum[:, t:t + 1], None, ALU.mult)
        k.dma('sp', cco[tsl, :].rearrange("(t p) e -> p t e", p=128), cc)
    k.finish()
    return nc


def run_C(inp, l, xT, mixT, mod_l):
    nc = build_C()
    ga, shf, scf = mod_l[2 * D:3 * D], mod_l[3 * D:4 * D], mod_l[4 * D:5 * D]
    f = lambda a: np.ascontiguousarray(a, dtype=np.float32)
    vecs = f(np.concatenate([col16(ga), col16(scf), col16(shf), col16(inp['norm_ffn'][l])], axis=1))
    wout_t = f(inp['w_out'][l].reshape(NCH, 128, 4, 512).transpose(2, 1, 0, 3).reshape(4, 128, 8192))
    wr = f(inp['router_w'][l].reshape(NCH, 128, 32).transpose(1, 0, 2).reshape(128, NCH * 32))
    rb = f(np.broadcast_to(inp['router_b'][l][None, :], (128, 32)))
    ones = np.ones((128, 128), NPBF)
    ident = np.eye(32, dtype=np.float32)
    in_maps = []
    for c in range(NCORES):
        tsl = slice(c * CTOK, (c + 1) * CTOK)
        in_maps.append({"xT": np.ascontiguousarray(xT[:, tsl]), "mixT": np.ascontiguousarray(mixT[:, tsl]),
                        "wout_t": wout_t, "vecs": vecs, "wr": wr, "rb": rb, "ones": ones, "ident": ident})
    res = run_bass_kernel_spmd(nc, in_maps, core_ids=list(range(NCORES)))
    x1 = np.concatenate([res.results[c]["x1T"].T for c in range(NCORES)], axis=0)
    hT = np.concatenate([res.results[c]["hT"] for c in range(NCORES)], axis=1)
    comb = np.concatenate([res.results[c]["cc"] for c in range(NCORES)], axis=0)
    return np.ascontiguousarray(x1), np.ascontiguousarray(hT), comb


def build_D(ng=NG):
    nc = bass.Bass("TRN2", target_bir_lowering=False)
    k = K(nc)
    St = ng * TG
    hTd = nc.dram_tensor("hT", [D, St], BF16, kind="ExternalInput").ap()
    cc_d = nc.dram_tensor("cc", [128, (St // 128) * NEL], F32, kind="ExternalInput").ap()
    wgu_in = nc.dram_tensor("wgu_bf", [NEL * 8, 128, 8192], BF16, kind="ExternalInput").ap()
    bgu_d = nc.dram_tensor("bgu", [128, NEL * 32], F32, kind="ExternalInput").ap()
    wd_in = nc.dram_tensor("wd_bf", [NEL * 4, 128, 8192], BF16, kind="ExternalInput").ap()
    bd_d = nc.dram_tensor("bd", [1, NEL * D], F32, kind="ExternalInput").ap()
    ones_d = nc.dram_tensor("ones", [128, 128], BF16, kind="ExternalInput").ap()
    y = nc.dram_tensor("y", [St, D], F32, kind="ExternalOutput").ap()
    wgu_bf = [[wgu_in[e * 8 + s_] for s_ in range(8)] for e in range(NEL)]
    wd_bf = [[wd_in[e * 4 + q] for q in range(4)] for e in range(NEL)]

    bgu = k.sb([128, NEL * 32], F32)
    ones = k.sb([128, 128], BF16)
    cc = k.sb([128, St // 128, NEL], F32)
    bd_sb = k.sb([1, NEL * D], BF16)
    k.dma('sp', bgu, bgu_d)
    k.dma('sp', ones, ones_d)
    k.dma('sp', cc, cc_d.rearrange("p (t e) -> p t e", e=NEL))
    for e in range(NEL):
        k.dma('pool', bd_sb[0:1, e * D:(e + 1) * D], bd_d[0:1, e * D:(e + 1) * D])

    hTs = [k.sb([128, NCH, TG], BF16) for _ in range(2)]
    actT = k.sb([128, NCH, TG], BF16)
    yaccs = [k.sb([128, 4, D], F32)] * 2
    slots = [k.sb([128, NCH, 512], BF16) for _ in range(4)]
    g1 = [k.sb([128, TG], F32) for _ in range(2)]
    sg = [k.sb([128, TG], F32) for _ in range(2)]
    u1 = [k.sb([128, TG], F32) for _ in range(2)]
    gs = [k.sb([128, TG], F32) for _ in range(2)]
    P4 = [k.ps() for _ in range(4)]
    Yb = [k.ps() for _ in range(4)]
    nslot = [0]
    ny = [0]

    def load_slot(src):
        sl = slots[nslot[0] % 4]
        nslot[0] += 1
        k.dma('sp', sl, src.rearrange("p (c n) -> p c n", n=512))
        return sl

    hTv = hTd.rearrange("(c p) t -> p c t", p=128)
    for g in range(ng):
        tsl = slice(g * TG, (g + 1) * TG)
        hT = hTs[g % 2]
        yacc = yaccs[g % 2]
        k.dma('sp', hT, hTv[:, :, tsl])
        for e in range(NEL):
            for s in range(8):
                sl = load_slot(wgu_bf[e][s])
                for pair in range(2):
                    j = 2 * s + pair
                    pp = j % 2
                    G = P4[2 * pp]
                    U = P4[2 * pp + 1]
                    for c in range(NCH):
                        k.mm(G, sl[:, c, pair * 128:(pair + 1) * 128], hT[:, c, :], start=(c == 0), stop=(c == NCH - 1))
                    for c in range(NCH):
                        k.mm(U, sl[:, c, 256 + pair * 128:256 + (pair + 1) * 128], hT[:, c, :], start=(c == 0), stop=(c == NCH - 1))
                    k.ts('dve', g1[pp], G, bgu[:, e * 32 + j:e * 32 + j + 1], 7.0, ALU.add, ALU.min)
                    k.act(sg[pp], g1[pp], AF.Sigmoid, scale=1.702)
                    k.ts('dve', u1[pp], U, bgu[:, e * 32 + 16 + j:e * 32 + 16 + j + 1], 7.0, ALU.add, ALU.min)
                    k.tt('pool', gs[pp], g1[pp], sg[pp], ALU.mult)
                    k.ts('dve', u1[pp], u1[pp], -7.0, 1.0, ALU.max, ALU.add)
                    k.tt('dve', actT[:, j, :], u1[pp], gs[pp], ALU.mult)
            for q in range(4):
                sl = load_slot(wd_bf[e][q])
                for t in range(4):
                    Y = Yb[ny[0] % 4]
                    ny[0] += 1
                    for j in range(NCH):
                        k.mm(Y, actT[:, j, t * 128:(t + 1) * 128], sl[:, j, :], start=(j == 0), stop=False)
                    k.mm(Y, ones[0:1, 0:128], bd_sb[0:1, e * D + q * 512:e * D + (q + 1) * 512], start=False, stop=True)
                    ysl = yacc[:, t, q * 512:(q + 1) * 512]
                    ccol = cc[:, 4 * g + t, e:e + 1]
                    if e == 0:
                        k.ts('dve', ysl, Y, ccol, None, ALU.mult)
                    else:
                        k.stt('dve', ysl, Y, ccol, ysl, ALU.mult, ALU.add)
        k.dma('sp', y[tsl, :].rearrange("(t p) d -> p t d", p=128), yacc)
    k.finish()
    return nc


def d_inputs(inp, l, hT, comb, c, wbf, ng=NG):
    St = ng * TG
    ex = [4 * c + i for i in range(NEL)]
    bgu_e = inp['b_gate_up'][l][ex]
    bg = bgu_e[:, 0::2].reshape(NEL, NCH, 128)
    bu = bgu_e[:, 1::2].reshape(NEL, NCH, 128)
    bgu = np.concatenate([bg, bu], axis=1).transpose(2, 0, 1).reshape(128, NEL * 32)
    bd = inp['b_down'][l][ex].reshape(1, NEL * D)
    ccl = comb[:St, 4 * c:4 * c + NEL].reshape(St // 128, 128, NEL).transpose(1, 0, 2).reshape(128, (St // 128) * NEL)
    f = lambda a: np.ascontiguousarray(a, dtype=np.float32)
    return {"hT": np.ascontiguousarray(hT[:, :St]), "cc": f(ccl), "wgu_bf": wbf[0], "bgu": f(bgu),
            "wd_bf": wbf[1], "bd": f(bd), "ones": np.ones((128, 128), NPBF)}


def run_D(inp, l, hT, comb, wbfs):
    nc = build_D()
    in_maps = [d_inputs(inp, l, hT, comb, c, wbfs[c]) for c in range(NCORES)]
    res = run_bass_kernel_spmd(nc, in_maps, core_ids=list(range(NCORES)))
    return [res.results[c]["y"] for c in range(NCORES)]


def build_E(final):
    nc = bass.Bass("TRN2", target_bir_lowering=False)
    k = K(nc)
    x1 = nc.dram_tensor("x1", [1024, D], F32, kind="ExternalInput").ap()
    yp = nc.dram_tensor("yp", [NCORES, 1024, D], F32, kind="ExternalInput").ap()
    gfb = nc.dram_tensor("gfb", [128, D], F32, kind="ExternalInput").ap()
    nfb = nc.dram_tensor("nfb", [128, D], F32, kind="ExternalInput").ap()
    out = nc.dram_tensor("out", [1024, D], F32, kind="ExternalOutput").ap()
    gf = k.sb([128, D], F32)
    nf = k.sb([128, D], F32)
    k.dma('sp', gf, gfb)
    k.dma('sp', nf, nfb)
    xt = [k.sb([128, D], F32) for _ in range(2)]
    yt = [k.sb([128, D], F32) for _ in range(4)]
    acc = [k.sb([128, D], F32) for _ in range(2)]
    junk = k.sb([128, D], F32)
    st = k.sb([128, 8], F32)
    ny = 0
    for t in range(8):
        rows = slice(t * 128, (t + 1) * 128)
        x_ = xt[t % 2]
        a_ = acc[t % 2]
        k.dma('sp', x_, x1[rows, :])
        for c in range(NCORES):
            yb = yt[ny % 4]
            ny += 1
            k.dma('sp', yb, yp[c, rows, :])
            if c == 0:
                k.copy('pool', a_, yb)
            else:
                k.tt('dve' if c % 2 else 'pool', a_, a_, yb, ALU.add)
        k.tt('dve', a_, a_, gf, ALU.mult)
        k.tt('dve', a_, a_, x_, ALU.add)
        if final:
            k.act(junk, a_, AF.Square, accum_out=st[:, t:t + 1])
            k.act(st[:, t:t + 1], st[:, t:t + 1], AF.Sqrt, bias=EPS, scale=1.0 / D)
            k.recip(st[:, t:t + 1], st[:, t:t + 1])
            k.stt('dve', a_, a_, st[:, t:t + 1], nf, ALU.mult, ALU.mult)
        k.dma('sp', out[rows, :], a_)
    k.finish()
    return nc


def run_E(inp, x1, yparts, gate_f, final):
    nc = build_E(final)
    gfb = np.ascontiguousarray(np.broadcast_to(gate_f[None, :], (128, D)).astype(np.float32))
    nfb = np.ascontiguousarray(np.broadcast_to(inp['norm_final'][None, :], (128, D)).astype(np.float32))
    in_maps = []
    for c in range(NCORES):
        rows = slice(c * 1024, (c + 1) * 1024)
        in_maps.append({"x1": np.ascontiguousarray(x1[rows]),
                        "yp": np.ascontiguousarray(np.stack([yparts[j][rows] for j in range(NCORES)])),
                        "gfb": gfb, "nfb": nfb})
    res = run_bass_kernel_spmd(nc, in_maps, core_ids=list(range(NCORES)))
    return np.concatenate([res.results[c]["out"] for c in range(NCORES)], axis=0)


def kernel(**inp):
    inp = {k_: np.asarray(v) for k_, v in inp.items()}
    mod = run_M(inp)
    x = np.ascontiguousarray(inp['x'][0], dtype=np.float32)
    for l in range(2):
        xT = np.ascontiguousarray(x.T)
        mixT, wbfs = run_AB(inp, l, xT, mod[l], cast_w=True)
        x1, hT, comb = run_C(inp, l, xT, mixT, mod[l])
        yparts = run_D(inp, l, hT, comb, wbfs)
        del wbfs
        x = run_E(inp, x1, yparts, mod[l][5 * D:6 * D], final=(l == 1))
    return x[None].astype(np.float32)
```
